# Optimizing a Trainium2 kernel written in Bass

```python
import jax
import jax.numpy as jnp
from jax import lax
import numpy as np


D_MODEL = 2048
BATCH = 1
SEQ = 8192
DEPTH = 4

GRID_W = 64
CTX_LEN = 256
N_MIXERS = 2
LRU_WIDTH = D_MODEL
LRU_BLOCKS = 8
LRU_BLOCK_DIM = LRU_WIDTH // LRU_BLOCKS
CONV_WIDTH = 4
LRU_C = 8.0
N_HEADS = 16
HEAD_DIM = D_MODEL // N_HEADS
WIN_H = 8
WIN_W = 16
N_EXPERTS = 16
EXPERT_FF = D_MODEL // 2
CAPACITY_FACTOR = 2
EPS = 1e-6

kernel_name = 'hybrid_rglru_natten_ecmoe_dit'


def rmsnorm(x, g):
    xf = x.astype(jnp.float32)
    y = xf * lax.rsqrt(jnp.mean(xf * xf, axis=-1, keepdims=True) + EPS)
    return (y * g.astype(jnp.float32)).astype(x.dtype)


def modulate(h, shift, scale):
    return h * (1 + scale) + shift


def dwconv_centred(x, w, b):
    n = x.shape[1]
    left = CONV_WIDTH // 2
    right = CONV_WIDTH - 1 - left
    xp = jnp.pad(x, ((0, 0), (left, right), (0, 0)))
    y = b
    for k in range(CONV_WIDTH):
        y = y + xp[:, k:k + n] * w[k]
    return y


def lru_coefficients(x, gate_w, gate_b, lam):
    B, n, W = x.shape
    xb = x.reshape(B, n, LRU_BLOCKS, LRU_BLOCK_DIM)
    g = jnp.einsum('bnkc,gkcd->gbnkd', xb, gate_w).reshape(2, B, n, W) + gate_b[:, None, None, :]
    g = g.astype(jnp.float32)
    r = jax.nn.sigmoid(g[0])
    i = jax.nn.sigmoid(g[1])
    log_a = -LRU_C * r * jax.nn.softplus(-lam.astype(jnp.float32))
    a = jnp.exp(log_a)
    mult = jnp.sqrt(jnp.maximum(-jnp.expm1(2.0 * log_a), 0.0))
    return a, mult * i * x.astype(jnp.float32)


def _combine(e1, e2):
    a1, b1 = e1
    a2, b2 = e2
    return a1 * a2, a2 * b1 + b2


def linear_scan(a, b, h0, reverse):
    if reverse:
        a = jnp.flip(a, axis=1)
        b = jnp.flip(b, axis=1)
    b = b.at[:, 0].add(a[:, 0] * h0)
    _, h = lax.associative_scan(_combine, (a, b), axis=1)
    if reverse:
        h = jnp.flip(h, axis=1)
        return h, h[:, 0]
    return h, h[:, -1]


def rglru_block(h_ctx, h_lat, w_in, conv_w, conv_b, gate_w, gate_b, lam, w_out, ctx_out):
    def branches(h):
        gate_br, rec = jnp.split(h @ w_in, 2, axis=-1)
        return jax.nn.gelu(gate_br), dwconv_centred(rec, conv_w, conv_b)

    g_c, x_c = branches(h_ctx)
    g_l, x_l = branches(h_lat)
    B = h_lat.shape[0]
    h0 = jnp.zeros((B, LRU_WIDTH), jnp.float32)
    hs_c = []
    hs_l = []
    for d in range(2):
        a_c, b_c = lru_coefficients(x_c, gate_w[d], gate_b[d], lam[d])
        h_c, h_c_final = linear_scan(a_c, b_c, h0, reverse=(d == 1))
        a_l, b_l = lru_coefficients(x_l, gate_w[d], gate_b[d], lam[d])
        h_l, _ = linear_scan(a_l, b_l, h_c_final, reverse=(d == 1))
        hs_c.append(h_c)
        hs_l.append(h_l)
    y_lat = (g_l * (hs_l[0] + hs_l[1]).astype(g_l.dtype)) @ w_out
    y_ctx = None
    if ctx_out:
        y_ctx = (g_c * (hs_c[0] + hs_c[1]).astype(g_c.dtype)) @ w_out
    return y_ctx, y_lat


def neighbourhood_attention(h_ctx, h_lat, w_qkv, rpb, w_out, ctx_out):
    B, n, _ = h_lat.shape
    L = h_ctx.shape[1]
    rows = n // GRID_W
    kh = min(WIN_H, rows)
    scale = HEAD_DIM ** -0.5
    qkv_l = (h_lat @ w_qkv).reshape(B, n, 3, N_HEADS, HEAD_DIM)
    qkv_c = (h_ctx @ w_qkv).reshape(B, L, 3, N_HEADS, HEAD_DIM)
    grid = (B, rows, GRID_W, N_HEADS, HEAD_DIM)
    q_grid = (qkv_l[:, :, 0] * scale).reshape(grid)
    k_grid = qkv_l[:, :, 1].reshape(grid)
    v_grid = qkv_l[:, :, 2].reshape(grid)
    q_c = qkv_c[:, :, 0] * scale
    k_c = qkv_c[:, :, 1]
    v_c = qkv_c[:, :, 2]
    cols = jnp.arange(GRID_W)
    col_start = jnp.clip(cols - WIN_W // 2, 0, GRID_W - WIN_W)
    col_idx = col_start[:, None] + jnp.arange(WIN_W)[None, :]
    rpb_cols = rpb[:, :, col_idx - cols[:, None] + (WIN_W - 1)]

    def row_block(r):
        rs = jnp.clip(r - kh // 2, 0, rows - kh)
        q_r = lax.dynamic_index_in_dim(q_grid, r, axis=1, keepdims=False)
        k_r = lax.dynamic_slice_in_dim(k_grid, rs, kh, axis=1)[:, :, col_idx]
        v_r = lax.dynamic_slice_in_dim(v_grid, rs, kh, axis=1)[:, :, col_idx]
        dr = rs + jnp.arange(kh) - r + (WIN_H - 1)
        bias = jnp.transpose(rpb_cols[:, dr], (0, 2, 1, 3))
        s_loc = jnp.einsum('bqhd,biqjhd->bhqij', q_r, k_r).astype(jnp.float32) + bias.astype(jnp.float32)
        s_ctx = jnp.einsum('bqhd,bkhd->bhqk', q_r, k_c).astype(jnp.float32)
        s = jnp.concatenate([s_loc.reshape(B, N_HEADS, GRID_W, kh * WIN_W), s_ctx], axis=-1)
        p = jax.nn.softmax(s, axis=-1).astype(v_r.dtype)
        p_loc = p[..., :kh * WIN_W].reshape(B, N_HEADS, GRID_W, kh, WIN_W)
        p_ctx = p[..., kh * WIN_W:]
        return jnp.einsum('bhqij,biqjhd->bqhd', p_loc, v_r) + jnp.einsum('bhqk,bkhd->bqhd', p_ctx, v_c)

    o = lax.map(row_block, jnp.arange(rows))
    y_lat = jnp.moveaxis(o, 0, 1).reshape(B, n, D_MODEL) @ w_out
    y_ctx = None
    if ctx_out:
        p_c = jax.nn.softmax(jnp.einsum('bqhd,bkhd->bhqk', q_c, k_c).astype(jnp.float32), axis=-1).astype(v_c.dtype)
        y_ctx = jnp.einsum('bhqk,bkhd->bqhd', p_c, v_c).reshape(B, L, D_MODEL) @ w_out
    return y_ctx, y_lat


def ec_moe(h, w_router, w_gate, w_up, w_down):
    B, n, D = h.shape
    cap = max(1, CAPACITY_FACTOR * n // N_EXPERTS)
    aff = jax.nn.softmax(jnp.einsum('bnd,de->bne', h, w_router).astype(jnp.float32), axis=-1)
    g, idx = lax.top_k(jnp.swapaxes(aff, 1, 2), cap)
    xs = jax.vmap(lambda hb, ib: hb[ib])(h, idx)
    u = jax.nn.silu(jnp.einsum('becd,edf->becf', xs, w_gate)) * jnp.einsum('becd,edf->becf', xs, w_up)
    y = jnp.einsum('becf,efd->becd', u, w_down) * g[..., None].astype(h.dtype)
    return jax.vmap(lambda ib, yb: jnp.zeros((n, D), yb.dtype).at[ib.reshape(-1)].add(yb.reshape(-1, D)))(idx, y)


def setup_inputs(seed: int = 0) -> dict:
    key = jax.random.key(seed)
    ks = jax.random.split(key, 24)
    n_a = (DEPTH + N_MIXERS - 1) // N_MIXERS
    n_b = DEPTH // N_MIXERS
    D, W, E, F = D_MODEL, LRU_WIDTH, N_EXPERTS, EXPERT_FF

    def nrm(k, shape, fan_in):
        return jax.random.normal(k, shape, jnp.float32) * fan_in ** -0.5

    a0 = jax.random.uniform(ks[13], (n_a, 2, W), jnp.float32, minval=0.9, maxval=0.999)
    p = a0 ** (1.0 / LRU_C)
    return {
        'x': jax.random.normal(ks[0], (BATCH, SEQ, D), jnp.float32),
        'c': jax.random.normal(ks[1], (BATCH, D), jnp.float32),
        'ctx': jax.random.normal(ks[2], (BATCH, CTX_LEN, D), jnp.float32),
        'c_ctx': jax.random.normal(ks[3], (D,), jnp.float32),
        'ada_w': nrm(ks[4], (DEPTH, D, 6 * D), D),
        'ada_b': 0.02 * jax.random.normal(ks[5], (DEPTH, 6 * D), jnp.float32),
        'norm_mix_g': 1.0 + 0.02 * jax.random.normal(ks[6], (DEPTH, D), jnp.float32),
        'norm_ffn_g': 1.0 + 0.02 * jax.random.normal(ks[7], (DEPTH, D), jnp.float32),
        'lru_w_in': nrm(ks[8], (n_a, D, 2 * W), D),
        'lru_conv_w': nrm(ks[9], (n_a, CONV_WIDTH, W), CONV_WIDTH),
        'lru_conv_b': 0.02 * jax.random.normal(ks[10], (n_a, W), jnp.float32),
        'lru_gate_w': nrm(ks[11], (n_a, 2, 2, LRU_BLOCKS, LRU_BLOCK_DIM, LRU_BLOCK_DIM), LRU_BLOCK_DIM),
        'lru_gate_b': 0.1 * jax.random.normal(ks[12], (n_a, 2, 2, W), jnp.float32),
        'lru_lambda': jnp.log(p) - jnp.log1p(-p),
        'lru_w_out': nrm(ks[14], (n_a, W, D), W),
        'attn_w_qkv': nrm(ks[15], (n_b, D, 3 * D), D),
        'attn_rpb': 0.1 * jax.random.normal(ks[16], (n_b, N_HEADS, 2 * WIN_H - 1, 2 * WIN_W - 1), jnp.float32),
        'attn_w_out': nrm(ks[17], (n_b, D, D), D),
        'moe_router': nrm(ks[18], (DEPTH, D, E), D),
        'moe_w_gate': nrm(ks[19], (DEPTH, E, D, F), D),
        'moe_w_up': nrm(ks[20], (DEPTH, E, D, F), D),
        'moe_w_down': nrm(ks[21], (DEPTH, E, F, D), F),
        'final_norm_g': 1.0 + 0.02 * jax.random.normal(ks[22], (D,), jnp.float32),
    }


def reference(x, c, ctx, c_ctx, ada_w, ada_b, norm_mix_g, norm_ffn_g, lru_w_in, lru_conv_w, lru_conv_b,
              lru_gate_w, lru_gate_b, lru_lambda, lru_w_out, attn_w_qkv, attn_rpb, attn_w_out,
              moe_router, moe_w_gate, moe_w_up, moe_w_down, final_norm_g):
    for i in range(DEPTH):
        last = i == DEPTH - 1
        j = i // N_MIXERS
        sh1_l, sc1_l, g1_l, sh2_l, sc2_l, g2_l = jnp.split((jax.nn.silu(c) @ ada_w[i] + ada_b[i])[:, None, :], 6, axis=-1)
        sh1_c, sc1_c, g1_c, sh2_c, sc2_c, g2_c = jnp.split(jax.nn.silu(c_ctx) @ ada_w[i] + ada_b[i], 6, axis=-1)
        h_l = modulate(rmsnorm(x, norm_mix_g[i]), sh1_l, sc1_l)
        h_c = modulate(rmsnorm(ctx, norm_mix_g[i]), sh1_c, sc1_c)
        if i % N_MIXERS == 0:
            y_c, y_l = rglru_block(h_c, h_l, lru_w_in[j], lru_conv_w[j], lru_conv_b[j], lru_gate_w[j],
                                   lru_gate_b[j], lru_lambda[j], lru_w_out[j], ctx_out=not last)
        else:
            y_c, y_l = neighbourhood_attention(h_c, h_l, attn_w_qkv[j], attn_rpb[j], attn_w_out[j], ctx_out=not last)
        x = x + g1_l * y_l
        x = x + g2_l * ec_moe(modulate(rmsnorm(x, norm_ffn_g[i]), sh2_l, sc2_l),
                              moe_router[i], moe_w_gate[i], moe_w_up[i], moe_w_down[i])
        if not last:
            ctx = ctx + g1_c * y_c
            ctx = ctx + g2_c * ec_moe(modulate(rmsnorm(ctx, norm_ffn_g[i]), sh2_c, sc2_c),
                                      moe_router[i], moe_w_gate[i], moe_w_up[i], moe_w_down[i])
    return rmsnorm(x, final_norm_g)
```

```python
import contextlib
import numpy as np
import ml_dtypes
import concourse.bass as bass
import concourse.mybir as mybir
from concourse.bass_utils import run_bass_kernel_spmd

F32 = mybir.dt.float32
BF16 = mybir.dt.bfloat16
I32 = mybir.dt.int32
U32 = mybir.dt.uint32
AF = mybir.ActivationFunctionType
ALU = mybir.AluOpType
NPBF = ml_dtypes.bfloat16

NCORES = 8
D = 2048
KC = 16
SEQ = 8192
CTX = 256
NTOK = SEQ + CTX
TL = SEQ // NCORES
TC = CTX // NCORES
TT = TL + TC
DEPTH = 4
NE = 16
FF = 1024
CAPL = 1024
CAPC = 32
SLOTS = CAPL + CAPC
EPS = 1e-6
ENGS = ('sync', 'scalar', 'vector', 'gpsimd', 'tensor')


class Res:
    __slots__ = ('w', 'r', 'dkey', 'name')

    def __init__(self, name=''):
        self.w = None
        self.r = []
        self.dkey = None
        self.name = name


class Sched:
    def __init__(self, nc, stack):
        self.nc = nc
        self.stack = stack
        self.q = {e: [] for e in ENGS}
        self.esem = {}
        self.cnt = {e: 0 for e in ENGS}
        self.seen = {e: {} for e in ENGS}
        self.dsems = {}
        self.dcnt = {}
        self.finals = []

    def _semof(self, key):
        if key in ENGS:
            if key not in self.esem:
                self.esem[key] = self.stack.enter_context(self.nc.semaphore("es_" + key))
            return self.esem[key]
        return self.dsems[key]

    def _deps(self, eng, reads, writes):
        toks = []
        for r in reads:
            if r.w is not None:
                toks.append(r.w)
        for w in writes:
            if w.w is not None:
                toks.append(w.w)
            toks.extend(w.r)
        waits = []
        for (key, val) in toks:
            if eng == 'tensor' and key == 'tensor':
                continue
            if self.seen[eng].get(key, 0) >= val:
                continue
            self.seen[eng][key] = val
            waits.append((self._semof(key), val))
        return waits

    def _commit(self, tok, reads, writes):
        for r in reads:
            r.r.append(tok)
        for w in writes:
            w.w = tok
            w.r = []

    def op(self, eng, fn, reads=(), writes=()):
        waits = self._deps(eng, reads, writes)
        self.cnt[eng] += 1
        tok = (eng, self.cnt[eng])
        sem = self._semof(eng)

        def emit(e):
            for s, v in waits:
                e.wait_ge(s, v)
            fn(e).then_inc(sem, 1)
        self.q[eng].append(emit)
        self._commit(tok, reads, writes)
        return tok

    def dma(self, fns, reads=(), writes=(), eng='sync', final=False):
        if callable(fns):
            fns = [fns]
        waits = self._deps(eng, reads, writes)
        own = writes[0] if writes else reads[0]
        if own.dkey is None:
            own.dkey = ('d', len(self.dsems))
            self.dsems[own.dkey] = self.stack.enter_context(
                self.nc.semaphore("ds%d" % len(self.dsems)))
            self.dcnt[own.dkey] = 0
        key = own.dkey
        sem = self.dsems[key]
        self.dcnt[key] += 16 * len(fns)
        tok = (key, self.dcnt[key])

        def emit(e):
            for s, v in waits:
                e.wait_ge(s, v)
            for f in fns:
                f(e).then_inc(sem, 16)
        self.q[eng].append(emit)
        self._commit(tok, reads, writes)
        if final:
            self.finals.append(tok)
        return tok

    def emit_all(self):
        fin = [(self._semof(k), v) for (k, v) in self.finals]
        with self.nc.Block() as block:
            for eng in ENGS:
                if not self.q[eng] and eng != 'sync':
                    continue

                def body(e, eng=eng):
                    for f in self.q[eng]:
                        f(e)
                    if eng == 'sync':
                        for s, v in fin:
                            e.wait_ge(s, v)
                getattr(block, eng)(body)


class Ctx:
    def __init__(self):
        self.nc = bass.Bass("TRN2", target_bir_lowering=False)
        self.stack = contextlib.ExitStack()
        self.s = Sched(self.nc, self.stack)
        self.n = 0

    def dram_in(self, name, shape, dt):
        return self.nc.dram_tensor(name, list(shape), dt, kind="ExternalInput").ap()

    def dram_out(self, name, shape, dt):
        return self.nc.dram_tensor(name, list(shape), dt, kind="ExternalOutput").ap()

    def sb(self, shape, dt, name=None):
        self.n += 1
        return self.stack.enter_context(
            self.nc.sbuf_tensor(name or ("t%d" % self.n), list(shape), dt))

    def ps(self, shape, dt=F32, name=None):
        self.n += 1
        return self.stack.enter_context(
            self.nc.psum_tensor(name or ("p%d" % self.n), list(shape), dt))

    def breg(self, e, val):
        if not hasattr(self, '_bregs'):
            self._bregs = {}
        if val not in self._bregs:
            self._bregs[val] = e.to_reg(val)
        return self._bregs[val]

    def finish(self):
        self.s.emit_all()
        self.stack.close()
        return self.nc


def run(nc, in_maps):
    return run_bass_kernel_spmd(nc, in_maps, core_ids=list(range(NCORES))).results


ADA_COLS = 6 * D // NCORES
ADA_J = ADA_COLS // 128


def build_ada():
    c = Ctx()
    s = c.s
    cc = c.dram_in("cc", [128, KC, 2], F32)
    w = c.dram_in("w", [DEPTH, D, ADA_COLS], F32)
    b = c.dram_in("b", [128, DEPTH * ADA_J], F32)
    o = c.dram_out("o", [128, DEPTH * ADA_J, 2], F32)
    cct = c.sb([128, KC, 2], F32)
    sg = c.sb([128, KC, 2], F32)
    st = c.sb([128, KC, 2], F32)
    bt = c.sb([128, DEPTH * ADA_J], F32)
    ot = c.sb([128, DEPTH * ADA_J, 2], F32)
    wts = [c.sb([128, KC, 384], F32) for _ in range(2)]
    pss = [c.ps([128, 512]) for _ in range(2)]
    r_cc, r_st, r_b, r_o = Res(), Res(), Res(), Res()
    zb = c.sb([128, 128], BF16)
    r_zb = Res()
    s.op('vector', lambda e: e.memset(zb[:], 0.0), writes=[r_zb])
    r_w = [Res(), Res()]
    r_p = [Res(), Res()]
    s.dma(lambda e: e.dma_start(out=cct[:], in_=cc), writes=[r_cc])
    s.dma(lambda e: e.dma_start(out=bt[:], in_=b), writes=[r_b])
    r_sg = Res()
    s.op('scalar', lambda e: e.activation(out=sg[:], in_=cct[:], func=AF.Sigmoid), reads=[r_cc], writes=[r_sg])
    s.op('vector', lambda e: e.tensor_tensor(out=st[:], in0=sg[:], in1=cct[:], op=ALU.mult),
         reads=[r_sg, r_cc], writes=[r_st])
    n = 0
    pi = 0
    for l in range(DEPTH):
        wv = w[l].rearrange("(kc p) n -> p kc n", p=128)
        for g in range(4):
            wt = wts[n % 2]
            rw = r_w[n % 2]
            n += 1
            s.dma(lambda e, wt=wt, wv=wv, g=g: e.dma_start(out=wt[:], in_=wv[:, :, g * 384:(g + 1) * 384]),
                  writes=[rw])
            for j in range(3):
                ps = pss[pi % 2]
                rp = r_p[pi % 2]
                pi += 1
                idx = l * ADA_J + g * 3 + j
                for kc in range(KC):
                    s.op('tensor', lambda e, ps=ps, wt=wt, j=j, kc=kc: e.matmul(
                        ps[:, 0:2], lhsT=wt[:, kc, j * 128:(j + 1) * 128], rhs=st[:, kc, :],
                        start=(kc == 0), stop=(kc == KC - 1)),
                        reads=[rw, r_st], writes=[rp])
                s.op('tensor', lambda e, ps=ps: e.matmul(ps[:, 0:2], lhsT=zb[:], rhs=zb[:, 0:2], start=False, stop=True),
                     reads=[r_zb], writes=[rp])
                s.op('scalar', lambda e, ps=ps, idx=idx: e.activation(
                    out=ot[:, idx, :], in_=ps[:, 0:2], func=AF.Identity, bias=bt[:, idx:idx + 1], scale=1.0),
                    reads=[rp, r_b], writes=[r_o])
    s.dma(lambda e: e.dma_start(out=o, in_=ot[:]), reads=[r_o], final=True)
    return c.finish()


def run_ada(inp):
    nc = build_ada()
    cvec = np.stack([inp['c'][0], inp['c_ctx']], axis=-1).astype(np.float32)
    cc = np.ascontiguousarray(cvec.reshape(KC, 128, 2).transpose(1, 0, 2))
    maps = []
    for k in range(NCORES):
        wk = np.ascontiguousarray(inp['ada_w'][:, :, k * ADA_COLS:(k + 1) * ADA_COLS])
        bk = inp['ada_b'][:, k * ADA_COLS:(k + 1) * ADA_COLS].reshape(DEPTH, ADA_J, 128)
        bk = np.ascontiguousarray(bk.transpose(2, 0, 1).reshape(128, DEPTH * ADA_J))
        maps.append({"cc": cc, "w": wk, "b": bk})
    res = run(nc, maps)
    mods = np.zeros((DEPTH, 2, 6 * D), np.float32)
    for k in range(NCORES):
        ok = res[k]["o"].reshape(128, DEPTH, ADA_J, 2)
        mods[:, :, k * ADA_COLS:(k + 1) * ADA_COLS] = ok.transpose(1, 3, 2, 0).reshape(DEPTH, 2, ADA_COLS)
    return mods


def fm(v):
    return np.ascontiguousarray(np.asarray(v, np.float32).reshape(KC, 128).T)


BLOCKS = [(0, 512, 'l'), (512, 1024, 'l'), (1024, 1056, 'c')]
NTILE = 9
YROWS = NE * SLOTS


def emit_rstd(c, s, X, r_X, ones, r_ones, sqs, r_sq, S, r_S, rstd, r_rstd, lo, hi):
    w = hi - lo
    for kc in range(KC):
        sq = sqs[kc % 2]
        rq = r_sq[kc % 2]
        s.op('scalar', lambda e, sq=sq, kc=kc: e.activation(out=sq[:, :w], in_=X[:, kc, lo:hi], func=AF.Square),
             reads=[r_X[kc] if isinstance(r_X, list) else r_X], writes=[rq])
        s.op('tensor', lambda e, sq=sq, kc=kc: e.matmul(S[:, :w], lhsT=c.onesb[:], rhs=sq[:, :w],
                                                       start=(kc == 0), stop=(kc == KC - 1)),
             reads=[rq, r_ones], writes=[r_S])
    s.op('scalar', lambda e: e.activation(out=rstd[:, :w], in_=S[:, :w], func=AF.Sqrt, bias=c.epsb[:, 0:1], scale=1.0 / D),
         reads=[r_S, r_ones], writes=[r_rstd])
    s.op('vector', lambda e: e.reciprocal(out=rstd[:, :w], in_=rstd[:, :w]), reads=[r_rstd], writes=[r_rstd])


def build_t1(mode, combine):
    c = Ctx()
    s = c.s
    x = c.dram_in("x", [D, TT], F32)
    vec = c.dram_in("vec", [128, 8, KC], F32)
    X = c.sb([128, KC, TT], F32)
    V = c.sb([128, 8, KC], F32)
    ones = c.sb([128, 128], F32)
    c.epsb = c.sb([128, 1], F32)
    sqs = [c.sb([128, 512], BF16) for _ in range(2)]
    c.onesb = c.sb([128, 128], BF16)
    tmps = [c.sb([128, 512], F32) for _ in range(2)]
    rstd = c.sb([128, 512], F32)
    gs = c.sb([128, 2, KC], F32)
    S = c.ps([128, 512])
    r_X, r_V, r_ones, r_S, r_rstd, r_gs = Res(), Res(), Res(), Res(), Res(), Res()
    r_sq = [Res(), Res()]
    r_tmp = [Res(), Res()]
    s.op('vector', lambda e: e.memset(ones[:], 1.0), writes=[r_ones])
    s.op('vector', lambda e: e.memset(c.epsb[:], EPS), writes=[r_ones])
    s.op('vector', lambda e: e.memset(c.onesb[:], 1.0), writes=[r_ones])
    xv = x.rearrange("(kc p) n -> p kc n", p=128)
    s.dma([lambda e, q=q: e.dma_start(out=X[:, 4 * q:4 * q + 4, :], in_=xv[:, 4 * q:4 * q + 4, :]) for q in range(4)],
          writes=[r_X])
    s.dma(lambda e: e.dma_start(out=V[:], in_=vec), writes=[r_V])
    for wi, row in ((0, 2), (1, 4)):
        s.op('vector', lambda e, wi=wi, row=row: e.scalar_tensor_tensor(
            out=gs[:, wi, :], in0=V[:, row, :], scalar=1.0, in1=V[:, 0, :], op0=ALU.add, op1=ALU.mult),
            reads=[r_V], writes=[r_gs])

    if combine:
        yall = c.dram_in("yall", [YROWS, D], BF16)
        ridx = c.dram_in("ridx", [128, NTILE, NE], I32)
        gsel = c.dram_in("afft", [128, NTILE, NE], F32)
        RI = c.sb([128, NTILE, NE], I32)
        GS = c.sb([128, NTILE, NE], F32)
        identb_d = c.dram_in("identb", [128, 128], BF16)
        identb = c.sb([128, 128], BF16)
        r_idb = Res()
        s.dma(lambda e: e.dma_start(out=identb[:], in_=identb_d), writes=[r_idb])
        r_RI, r_GS = Res(), Res()
        s.dma(lambda e: e.dma_start(out=RI[:], in_=ridx), writes=[r_RI])
        s.dma(lambda e: e.dma_start(out=GS[:], in_=gsel), writes=[r_GS])
        MSK = c.sb([128, NTILE, NE], F32)
        r_MSK = Res()
        s.op('vector', lambda e: e.tensor_scalar(out=MSK[:], in0=RI[:], scalar1=BIG / 2, scalar2=None, op0=ALU.is_lt),
             reads=[r_RI], writes=[r_MSK])
        s.op('vector', lambda e: e.tensor_tensor(out=GS[:], in0=GS[:], in1=MSK[:], op=ALU.mult),
             reads=[r_MSK, r_GS], writes=[r_GS])
        GBS = [[c.sb([128, D], BF16) for _ in range(8)] for _ in range(2)]
        DGT = [c.sb([128, 8, 128], BF16) for _ in range(2)]
        PB = [[S] + [c.ps([128, 512]) for _ in range(3)], [c.ps([128, 512]) for _ in range(4)]]
        r_GBS = [[Res() for _ in range(8)] for _ in range(2)]
        r_DGT = [Res(), Res()]
        r_PB = [[r_S] + [Res() for _ in range(3)], [Res() for _ in range(4)]]
        for st_ in range(2):
            for b8 in range(8):
                s.op('vector', lambda e, st_=st_, b8=b8: e.memset(GBS[st_][b8][:], 0.0), writes=[r_GBS[st_][b8]])
        hn = 0
        for j in range(NTILE):
            nt = 128 if j < 8 else TC
            c0 = j * 128
            g2row = 5 if j < 8 else 6
            for hf in range(2):
                st_ = hn % 2
                hn += 1
                for e8 in range(8):
                    ex = hf * 8 + e8
                    gb = GBS[st_][e8]
                    s.dma(lambda e, gb=gb, j=j, ex=ex: e.indirect_dma_start(
                        out=gb[:], out_offset=None, in_=yall,
                        in_offset=bass.IndirectOffsetOnAxis(ap=RI[:, j, ex:ex + 1], axis=0),
                        bounds_check=c.breg(e, YROWS - 1), oob_is_err=False),
                        reads=[r_RI], writes=[r_GBS[st_][e8]], eng='gpsimd')
                    s.op('vector', lambda e, st_=st_, e8=e8, j=j, ex=ex: e.tensor_scalar(
                        out=DGT[st_][:, e8, :], in0=identb[:], scalar1=GS[:, j, ex:ex + 1], scalar2=None, op0=ALU.mult),
                        reads=[r_idb, r_GS], writes=[r_DGT[st_]])
                for fc in range(KC):
                    pb, rpb = PB[st_][fc // 4], r_PB[st_][fc // 4]
                    for e8 in range(8):
                        s.op('tensor', lambda e, pb=pb, st_=st_, fc=fc, e8=e8: e.matmul(
                            pb[:, (fc % 4) * 128:(fc % 4 + 1) * 128], lhsT=GBS[st_][e8][:, fc * 128:(fc + 1) * 128],
                            rhs=DGT[st_][:, e8, :], start=(e8 == 0), stop=(e8 == 7)),
                            reads=[r_GBS[st_][e8], r_DGT[st_]], writes=[rpb])
                for fc in range(KC):
                    pb, rpb = PB[st_][fc // 4], r_PB[st_][fc // 4]
                    s.op('vector', lambda e, pb=pb, fc=fc, nt=nt, c0=c0, g2row=g2row: e.scalar_tensor_tensor(
                        out=X[:, fc, c0:c0 + nt], in0=pb[:, (fc % 4) * 128:(fc % 4) * 128 + nt],
                        scalar=V[:, g2row, fc:fc + 1], in1=X[:, fc, c0:c0 + nt], op0=ALU.mult, op1=ALU.add),
                        reads=[rpb, r_V, r_X], writes=[r_X])
        if mode == 'norm':
            xo = c.dram_out("xo", [D, TT], F32)
            xov = xo.rearrange("(kc p) n -> p kc n", p=128)
            s.dma([lambda e, q=q: e.dma_start(out=xov[:, 4 * q:4 * q + 4, :], in_=X[:, 4 * q:4 * q + 4, :])
                   for q in range(4)], reads=[r_X], final=True)

    if mode == 'norm':
        h = c.dram_out("h", [D, TT], BF16)
        H = c.sb([128, KC, TT], BF16)
        r_H = Res()
        ti = 0
        for (lo, hi, which) in BLOCKS:
            w = hi - lo
            emit_rstd(c, s, X, r_X, ones, r_ones, sqs, r_sq, S, r_S, rstd, r_rstd, lo, hi)
            wi = 0 if which == 'l' else 1
            shrow = 1 if which == 'l' else 3
            for kc in range(KC):
                tmp = tmps[ti % 2]
                rt = r_tmp[ti % 2]
                ti += 1
                s.op('vector', lambda e, tmp=tmp, kc=kc, wi=wi, lo=lo, hi=hi, w=w: e.scalar_tensor_tensor(
                    out=tmp[:, :w], in0=X[:, kc, lo:hi], scalar=gs[:, wi, kc:kc + 1], in1=rstd[:, :w],
                    op0=ALU.mult, op1=ALU.mult), reads=[r_X, r_gs, r_rstd], writes=[rt])
                s.op('scalar', lambda e, tmp=tmp, kc=kc, shrow=shrow, lo=lo, hi=hi, w=w: e.activation(
                    out=H[:, kc, lo:hi], in_=tmp[:, :w], func=AF.Identity, bias=V[:, shrow, kc:kc + 1], scale=1.0),
                    reads=[rt, r_V], writes=[r_H])
        hv = h.rearrange("(kc p) n -> p kc n", p=128)
        s.dma([lambda e, q=q: e.dma_start(out=hv[:, 4 * q:4 * q + 4, :], in_=H[:, 4 * q:4 * q + 4, :])
               for q in range(4)], reads=[r_H], final=True)
    else:
        o = c.dram_out("o", [D, TL], F32)
        OB = [c.sb([128, KC, 512], F32)]
        r_OB = [Res()]
        ov = o.rearrange("(kc p) n -> p kc n", p=128)
        for bi_, (lo, hi, which) in enumerate(BLOCKS[:2]):
            w = hi - lo
            O, r_O = OB[0], r_OB[0]
            emit_rstd(c, s, X, r_X, ones, r_ones, sqs, r_sq, S, r_S, rstd, r_rstd, lo, hi)
            for kc in range(KC):
                s.op('vector', lambda e, kc=kc, lo=lo, hi=hi, w=w, O=O: e.scalar_tensor_tensor(
                    out=O[:, kc, :w], in0=X[:, kc, lo:hi], scalar=V[:, 0, kc:kc + 1], in1=rstd[:, :w],
                    op0=ALU.mult, op1=ALU.mult), reads=[r_X, r_V, r_rstd], writes=[r_O])
            s.dma([lambda e, q=q, lo=lo, hi=hi, O=O: e.dma_start(out=ov[:, 4 * q:4 * q + 4, lo:hi], in_=O[:, 4 * q:4 * q + 4, :])
                   for q in range(4)], reads=[r_O], final=True)
    return c.finish()


def tile_wout(w):
    return np.ascontiguousarray(w.reshape(KC, 128, KC, 128).transpose(2, 1, 0, 3).reshape(KC, 128, KC * 128))


def pack_vec(rows):
    v = np.zeros((128, 8, KC), np.float32)
    for i, r in rows.items():
        v[:, i, :] = fm(r)
    return v


TBLK = [(0, 256)] + [(256 + 512 * i, 256 + 512 * (i + 1)) for i in range(16)]
RECW = NTOK + 5


def rec_col(c0):
    return c0 + (2 if c0 < 256 else 4)


def build_lru():
    c = Ctx()
    s = c.s
    h = c.dram_in("h", [D, NTOK], BF16)
    win = c.dram_in("win", [D, 512], F32)
    cw_d = c.dram_in("cw", [128, 2, 8], F32)
    gw_d = c.dram_in("gw", [2, 2, 256, 256], F32)
    gv_d = c.dram_in("gv", [128, 2, 8], F32)
    z = c.dram_out("z", [256, NTOK], BF16)

    G = c.sb([128, 2, NTOK], BF16)
    REC = c.sb([128, 2, RECW], F32)
    Hs = c.sb([128, 2, NTOK], BF16)
    STG = [c.sb([128, 2, 512], F32)] * 2
    AR = c.sb([128, 13312], F32)
    GVH = c.sb([128, 2, 4], F32)
    r_GVH = Res()
    WB = AR[:, 8192:12288].bitcast(BF16).rearrange("p (k n) -> p k n", k=KC)
    HT = [AR[:, 0:4096].bitcast(BF16).rearrange("p (k n) -> p k n", k=KC),
          AR[:, 4096:8192].bitcast(BF16).rearrange("p (k n) -> p k n", k=KC)]
    CW = c.sb([128, 2, 8], F32)
    GV = c.sb([128, 2, 8], F32)
    GWS = c.sb([128, 2, 256], F32)
    GWB = c.sb([128, 4, 2, 256], BF16)
    CF = c.sb([128, 2, 16], F32)
    onec = c.sb([128, 1], F32)
    PS = [c.ps([128, 512]) for _ in range(8)]
    r_PS = [Res() for _ in range(8)]

    r_G, r_REC, r_H, r_WB, r_CW, r_GV, r_GWB, r_CF, r_one = (Res() for _ in range(9))
    r_STG = [Res()] * 2
    r_HT = [Res(), Res()]
    r_GWS = Res()

    s.dma(lambda e: e.dma_start(out=CW[:], in_=cw_d), writes=[r_CW])
    s.dma(lambda e: e.dma_start(out=GV[:], in_=gv_d), writes=[r_GV])
    s.op('vector', lambda e: e.tensor_scalar(out=GVH[:], in0=GV[:, :, 0:4], scalar1=0.5, scalar2=None, op0=ALU.mult),
         reads=[r_GV], writes=[r_GVH])
    s.op('vector', lambda e: e.memset(onec[:], 0.25), writes=[r_one])
    for (a, b) in ((0, 2), (258, 260), (RECW - 1, RECW)):
        s.op('vector', lambda e, a=a, b=b: e.memset(REC[:, :, a:b], 0.0), writes=[r_REC])

    r_WBq = [Res() for _ in range(8)]
    wv = win.rearrange("(kc p) n -> p kc n", p=128)
    for q in range(8):
        st = STG[q % 2]
        rs_ = r_STG[q % 2]
        s.dma(lambda e, st=st, q=q: e.dma_start(out=st[:], in_=wv[:, 2 * q:2 * q + 2, :]), writes=[rs_])
        s.op('scalar', lambda e, st=st, q=q: e.activation(out=WB[:, 2 * q:2 * q + 2, :], in_=st[:], func=AF.Copy),
             reads=[rs_], writes=[r_WBq[q]])
    for dg in range(4):
        s.dma(lambda e, dg=dg: e.dma_start(out=GWS[:], in_=gw_d[dg // 2, dg % 2].rearrange("(kc p) n -> p kc n", p=128)),
              writes=[r_GWS])
        s.op('gpsimd', lambda e, dg=dg: e.tensor_copy(out=GWB[:, dg, :, :], in_=GWS[:]), reads=[r_GWS], writes=[r_GWB])

    lam = GV[:, :, 4:6]
    T = [c.sb([128, 2, 2], F32) for _ in range(6)]
    r_T = Res()
    tal, tx, tsv, ts2, tp, trl = T

    def vop(fn, reads=(), writes=()):
        return s.op('vector', fn, reads=[r_T, r_GV] + list(reads), writes=[r_T] + list(writes))
    vop(lambda e: e.tensor_scalar(out=tal[:], in0=lam, scalar1=-1.0, scalar2=None, op0=ALU.mult))
    vop(lambda e: e.tensor_tensor(out=tal[:], in0=tal[:], in1=lam, op=ALU.max))
    s.op('scalar', lambda e: e.activation(out=tx[:], in_=tal[:], func=AF.Exp, scale=-1.0), reads=[r_T], writes=[r_T])
    vop(lambda e: e.tensor_scalar(out=tsv[:], in0=tx[:], scalar1=2.0, scalar2=None, op0=ALU.add))
    vop(lambda e: e.reciprocal(out=tsv[:], in_=tsv[:]))
    vop(lambda e: e.tensor_tensor(out=tsv[:], in0=tsv[:], in1=tx[:], op=ALU.mult))
    vop(lambda e: e.tensor_tensor(out=ts2[:], in0=tsv[:], in1=tsv[:], op=ALU.mult))
    vop(lambda e: e.tensor_scalar(out=tp[:], in0=ts2[:], scalar1=1.0 / 13, scalar2=1.0 / 11, op0=ALU.mult, op1=ALU.add))
    for cf in (1.0 / 9, 1.0 / 7, 1.0 / 5, 1.0 / 3, 1.0):
        vop(lambda e: e.tensor_tensor(out=tp[:], in0=tp[:], in1=ts2[:], op=ALU.mult))
        vop(lambda e, cf=cf: e.tensor_scalar(out=tp[:], in0=tp[:], scalar1=cf, scalar2=None, op0=ALU.add))
    vop(lambda e: e.tensor_tensor(out=tp[:], in0=tp[:], in1=tsv[:], op=ALU.mult))
    vop(lambda e: e.tensor_scalar(out=trl[:], in0=lam, scalar1=-1.0, scalar2=0.0, op0=ALU.mult, op1=ALU.max))
    vop(lambda e: e.scalar_tensor_tensor(out=trl[:], in0=tp[:], scalar=2.0, in1=trl[:], op0=ALU.mult, op1=ALU.add))
    vop(lambda e: e.tensor_scalar(out=CF[:, :, 0:2], in0=trl[:], scalar1=-4.0, scalar2=None, op0=ALU.mult), writes=[r_CF])
    vop(lambda e: e.tensor_scalar(out=CF[:, :, 2:4], in0=trl[:], scalar1=-8.0, scalar2=None, op0=ALU.mult), writes=[r_CF])

    hv = h.rearrange("(kc p) n -> p kc n", p=128)
    pi = 0
    last_pe = None
    for bi, (c0, c1) in enumerate(TBLK):
        w = c1 - c0
        ht = HT[bi % 2]
        rht = r_HT[bi % 2]
        s.dma([lambda e, ht=ht, c0=c0, c1=c1, w=w, q=q: e.dma_start(out=ht[:, 4 * q:4 * q + 4, :w], in_=hv[:, 4 * q:4 * q + 4, c0:c1])
               for q in range(4)], writes=[rht])
        for m in range(4):
            ps = PS[pi % 4]
            rp = r_PS[pi % 4]
            pi += 1
            for kc in range(KC):
                last_pe = s.op('tensor', lambda e, ps=ps, ht=ht, m=m, kc=kc, w=w: e.matmul(
                    ps[:, :w], lhsT=WB[:, kc, m * 128:(m + 1) * 128], rhs=ht[:, kc, :w],
                    start=(kc == 0), stop=(kc == KC - 1)), reads=[r_WBq[kc // 2], rht], writes=[rp])
            if m < 2:
                s.op('scalar', lambda e, ps=ps, m=m, c0=c0, c1=c1, w=w: e.activation(
                    out=G[:, m, c0:c1], in_=ps[:, :w], func=AF.Gelu_apprx_tanh), reads=[rp], writes=[r_G])
            else:
                rc = rec_col(c0)
                s.op('vector', lambda e, ps=ps, m=m, rc=rc, w=w: e.tensor_copy(
                    out=REC[:, m - 2, rc:rc + w], in_=ps[:, :w]), reads=[rp], writes=[r_REC])

    def tmp(i, dt=F32):
        ap = AR[:, 512 * i:512 * (i + 1)]
        return ap if dt == F32 else ap.bitcast(BF16)
    ti = [0]

    def nt(dt=F32):
        ti[0] += 1
        return tmp(ti[0] - 1, dt)

    def guard():
        r = Res()
        r.r = [last_pe]
        return r
    XC = [[nt(), nt()] for _ in range(2)]
    xcbt = [nt(BF16) for _ in range(2)]
    XCB = [[xcbt[p][:, 0:512], xcbt[p][:, 512:1024]] for p in range(2)]
    THR = [[nt(), nt()] for _ in range(2)]
    THI = [[nt(), nt()] for _ in range(2)]
    AA = [[nt(), nt()] for _ in range(2)]
    HB = [[nt(), nt()] for _ in range(2)]
    zbt = [nt(BF16), nt(BF16)]
    ZB = [zbt[p][:, 0:1024].rearrange("p (k n) -> p k n", k=2) for p in range(2)]
    TS = [nt(), nt()]
    assert ti[0] * 512 <= 13312
    r_XC = [[guard(), guard()] for _ in range(2)]
    r_XCB = [[guard(), guard()] for _ in range(2)]
    r_THR = [[guard(), guard()] for _ in range(2)]
    r_THI = [[guard(), guard()] for _ in range(2)]
    r_AA = [[guard(), guard()] for _ in range(2)]
    r_HB = [[guard(), guard()] for _ in range(2)]
    r_ZB = [guard(), guard()]
    r_TS = [guard(), guard()]
    psn = [0]

    def nextps():
        i = psn[0] % 8
        psn[0] += 1
        return PS[i], r_PS[i]

    def stage_a(d, bi, par):
        c0, c1 = TBLK[bi]
        w = c1 - c0
        rc = rec_col(c0)
        for ch in range(2):
            s.op('scalar', lambda e, ch=ch: e.activation(
                out=XC[par][ch][:, :w], in_=REC[:, ch, rc - 2:rc - 2 + w], func=AF.Identity,
                scale=CW[:, ch, 0:1], bias=CW[:, ch, 4:5]), reads=[r_REC, r_CW], writes=[r_XC[par][ch]])
            for k in (1, 2, 3):
                s.op('vector', lambda e, ch=ch, k=k: e.scalar_tensor_tensor(
                    out=XC[par][ch][:, :w], in0=REC[:, ch, rc - 2 + k:rc - 2 + k + w], scalar=CW[:, ch, k:k + 1],
                    in1=XC[par][ch][:, :w], op0=ALU.mult, op1=ALU.add),
                    reads=[r_REC, r_CW, r_XC[par][ch]], writes=[r_XC[par][ch]])
            s.op('scalar', lambda e, ch=ch: e.activation(
                out=XCB[par][ch][:, :w], in_=XC[par][ch][:, :w], func=AF.Identity),
                reads=[r_XC[par][ch]], writes=[r_XCB[par][ch]])
        pst = {}
        for g in range(2):
            for m in range(2):
                ps, rp = nextps()
                pst[(g, m)] = (ps, rp)
                for kc in range(2):
                    s.op('tensor', lambda e, ps=ps, g=g, m=m, kc=kc: e.matmul(
                        ps[:, :w], lhsT=GWB[:, d * 2 + g, kc, m * 128:(m + 1) * 128], rhs=XCB[par][kc][:, :w],
                        start=(kc == 0), stop=(kc == 1)), reads=[r_GWB, r_XCB[par][kc]], writes=[rp])
        for m in range(2):
            ps, rp = pst[(0, m)]
            s.op('scalar', lambda e, ps=ps, m=m: e.activation(
                out=THR[par][m][:, :w], in_=ps[:, :w], func=AF.Tanh, bias=GVH[:, m, 2 * d:2 * d + 1], scale=0.5),
                reads=[rp, r_GVH], writes=[r_THR[par][m]])
            ps, rp = pst[(1, m)]
            s.op('scalar', lambda e, ps=ps, m=m: e.activation(
                out=THI[par][m][:, :w], in_=ps[:, :w], func=AF.Tanh, bias=GVH[:, m, 2 * d + 1:2 * d + 2], scale=0.5),
                reads=[rp, r_GVH], writes=[r_THI[par][m]])
        for m in range(2):
            s.op('scalar', lambda e, m=m: e.activation(
                out=AA[par][m][:, :w], in_=THR[par][m][:, :w], func=AF.Exp, scale=CF[:, m, d:d + 1], bias=CF[:, m, d:d + 1]),
                reads=[r_THR[par][m], r_CF], writes=[r_AA[par][m]])
            s.op('scalar', lambda e, m=m: e.activation(
                out=THR[par][m][:, :w], in_=THR[par][m][:, :w], func=AF.Exp, scale=CF[:, m, 2 + d:3 + d], bias=CF[:, m, 2 + d:3 + d]),
                reads=[r_THR[par][m], r_CF], writes=[r_THR[par][m]])

    def stage_b(d, bi, par, first, lastw_):
        c0, c1 = TBLK[bi]
        w = c1 - c0
        for m in range(2):
            s.op('vector', lambda e, m=m: e.tensor_scalar(
                out=THR[par][m][:, :w], in0=THR[par][m][:, :w], scalar1=1.0, scalar2=None, op0=ALU.min),
                reads=[r_THR[par][m]], writes=[r_THR[par][m]])
        for m in range(2):
            s.op('scalar', lambda e, m=m: e.activation(
                out=THR[par][m][:, :w], in_=THR[par][m][:, :w], func=AF.Sqrt, bias=onec[:, 0:1], scale=-0.25),
                reads=[r_THR[par][m], r_one], writes=[r_THR[par][m]])

    def stage_b2(d, bi, par, first, lastw_):
        c0, c1 = TBLK[bi]
        w = c1 - c0
        for m in range(2):
            s.op('vector', lambda e, m=m: e.scalar_tensor_tensor(
                out=THR[par][m][:, :w], in0=THI[par][m][:, :w], scalar=1.0, in1=THR[par][m][:, :w],
                op0=ALU.add, op1=ALU.mult), reads=[r_THR[par][m], r_THI[par][m]], writes=[r_THR[par][m]])
            s.op('vector', lambda e, m=m: e.tensor_tensor(
                out=THR[par][m][:, :w], in0=THR[par][m][:, :w], in1=XC[par][m][:, :w], op=ALU.mult),
                reads=[r_THR[par][m], r_XC[par][m]], writes=[r_THR[par][m]])
            hb = HB[par][m]
            hprev = HB[1 - par][m]
            if d == 0:
                init = 0.0 if first else hprev[:, lastw_ - 1:lastw_]
                s.op('vector', lambda e, m=m, hb=hb, init=init: e.tensor_tensor_scan(
                    out=hb[:, :w], data0=AA[par][m][:, :w], data1=THR[par][m][:, :w], initial=init, op0=ALU.mult, op1=ALU.add),
                    reads=[r_AA[par][m], r_THR[par][m], r_HB[1 - par][m]], writes=[r_HB[par][m]])
                s.op('gpsimd', lambda e, m=m, hb=hb: e.tensor_copy(out=Hs[:, m, c0:c1], in_=hb[:, :w]),
                     reads=[r_HB[par][m]], writes=[r_H])
            else:
                init = 0.0 if first else hprev[:, 0:1]
                s.op('vector', lambda e, m=m, hb=hb, init=init: e.tensor_tensor_scan(
                    out=hb[:, :w][:, ::-1], data0=AA[par][m][:, :w][:, ::-1], data1=THR[par][m][:, :w][:, ::-1],
                    initial=init, op0=ALU.mult, op1=ALU.add),
                    reads=[r_AA[par][m], r_THR[par][m], r_HB[1 - par][m]], writes=[r_HB[par][m]])
                zb = ZB[par]
                s.op('gpsimd', lambda e, m=m, hb=hb: e.tensor_tensor(
                    out=TS[par][:, :w], in0=hb[:, :w], in1=Hs[:, m, c0:c1], op=ALU.add),
                    reads=[r_HB[par][m], r_H, r_TS[par]], writes=[r_TS[par]])
                s.op('gpsimd', lambda e, m=m, zb=zb: e.tensor_tensor(
                    out=zb[:, m, :w], in0=TS[par][:, :w], in1=G[:, m, c0:c1], op=ALU.mult),
                    reads=[r_TS[par], r_G], writes=[r_ZB[par]])
        if d == 1:
            zb = ZB[par]
            s.dma([lambda e, zb=zb, m=m: e.dma_start(out=z[m * 128:(m + 1) * 128, c0:c1], in_=zb[:, m, :w]) for m in range(2)],
                  reads=[r_ZB[par]], final=True)

    seq = [(0, bi) for bi in range(17)] + [(1, bi) for bi in [0] + list(range(16, 0, -1))]
    stage_a(seq[0][0], seq[0][1], 0)
    for n, (d, bi) in enumerate(seq):
        first = (n == 0) or (n == 17)
        pw = 0 if first else (TBLK[seq[n - 1][1]][1] - TBLK[seq[n - 1][1]][0])
        stage_b(d, bi, n % 2, first, pw)
        if n + 1 < len(seq):
            stage_a(seq[n + 1][0], seq[n + 1][1], (n + 1) % 2)
        stage_b2(d, bi, n % 2, first, pw)
    return c.finish()


def gather_h_all(hs):
    ctxp = np.concatenate([hk[:, TL:] for hk in hs], axis=1)
    latp = np.concatenate([hk[:, :TL] for hk in hs], axis=1)
    return np.ascontiguousarray(np.concatenate([ctxp, latp], axis=1))


def scatter_z(zs):
    zall = np.concatenate(zs, axis=0)
    out = []
    for k in range(NCORES):
        out.append(np.ascontiguousarray(np.concatenate(
            [zall[:, CTX + k * TL:CTX + (k + 1) * TL], zall[:, k * TC:(k + 1) * TC]], axis=1)))
    return out


def chan_vec(v, k):
    return np.asarray(v[k * 256:(k + 1) * 256], np.float32).reshape(2, 128).T


def lru_inputs(inp, j, k, h_all):
    w_in = inp['lru_w_in'][j]
    win = np.ascontiguousarray(np.concatenate([w_in[:, k * 256:(k + 1) * 256], w_in[:, D + k * 256:D + (k + 1) * 256]], axis=1))
    cw = np.zeros((128, 2, 8), np.float32)
    for t in range(4):
        cw[:, :, t] = chan_vec(inp['lru_conv_w'][j][t], k)
    cw[:, :, 4] = chan_vec(inp['lru_conv_b'][j], k)
    gv = np.zeros((128, 2, 8), np.float32)
    for d in range(2):
        for g in range(2):
            gv[:, :, 2 * d + g] = chan_vec(inp['lru_gate_b'][j][d][g], k)
        gv[:, :, 4 + d] = chan_vec(inp['lru_lambda'][j][d], k)
    gw = np.ascontiguousarray(inp['lru_gate_w'][j][:, :, k])
    return {"h": h_all, "win": win, "cw": cw, "gw": gw, "gv": gv}


def build_c():
    c = Ctx()
    s = c.s
    z = c.dram_in("z", [D, TT], BF16)
    x = c.dram_in("x", [D, TT], F32)
    wout = c.dram_in("wout", [KC, 128, KC * 128], F32)
    vec = c.dram_in("vec", [128, 8, KC], F32)
    wr_d = c.dram_in("wr", [128, KC, NE], F32)
    xo = c.dram_out("xo", [D, TT], F32)
    h2 = c.dram_out("h2", [D, TT], BF16)
    aff = c.dram_out("aff", [NE, TT], F32)

    X = c.sb([128, KC, TT], F32)
    Z = c.sb([128, KC, TT], BF16)
    V = c.sb([128, 8, KC], F32)
    WR = c.sb([128, KC, NE], F32)
    WS = [c.sb([128, KC, 128], F32) for _ in range(2)]
    WBF = [c.sb([128, KC, 128], BF16) for _ in range(2)]
    H2F = c.sb([128, KC, 512], F32)
    H2B = c.sb([128, KC, 512], BF16)
    ones = c.sb([128, 128], F32)
    c.epsb = c.sb([128, 1], F32)
    sqs = [c.sb([128, 512], BF16) for _ in range(2)]
    c.onesb = c.sb([128, 128], BF16)
    tmps = [c.sb([128, 512], F32) for _ in range(2)]
    rstd = c.sb([128, 512], F32)
    gs = c.sb([128, 2, KC], F32)
    E = c.sb([NE, 512], F32)
    RS = c.sb([NE, 512], F32)
    AFF = c.sb([NE, TT], F32)
    PS = [c.ps([128, 512]) for _ in range(4)]
    S = c.ps([128, 512])
    L = c.ps([128, 512])
    SM = c.ps([128, 512])
    r_PS = [Res() for _ in range(4)]
    r_Xm = [Res() for _ in range(KC)]
    r_Z, r_V, r_WR, r_ones, r_S, r_rstd, r_gs, r_L, r_SM, r_E, r_RS, r_AFF, r_H2F, r_H2B = (Res() for _ in range(14))
    r_WS = [Res(), Res()]
    r_WBF = [Res(), Res()]
    r_sq = [Res(), Res()]
    r_tmp = [Res(), Res()]

    s.op('vector', lambda e: e.memset(ones[:], 1.0), writes=[r_ones])
    s.op('vector', lambda e: e.memset(c.epsb[:], EPS), writes=[r_ones])
    s.op('vector', lambda e: e.memset(c.onesb[:], 1.0), writes=[r_ones])
    zerob = c.sb([128, 128], BF16)
    s.op('vector', lambda e: e.memset(zerob[:], 0.0), writes=[r_ones])
    zv = z.rearrange("(kc p) n -> p kc n", p=128)
    xv = x.rearrange("(kc p) n -> p kc n", p=128)
    s.dma([lambda e, q=q: e.dma_start(out=Z[:, 4 * q:4 * q + 4, :], in_=zv[:, 4 * q:4 * q + 4, :]) for q in range(4)],
          writes=[r_Z])
    s.dma(lambda e: e.dma_start(out=V[:], in_=vec), writes=[r_V])
    s.dma(lambda e: e.dma_start(out=WR[:], in_=wr_d), writes=[r_WR])
    for wi, row in ((0, 2), (1, 4)):
        s.op('vector', lambda e, wi=wi, row=row: e.scalar_tensor_tensor(
            out=gs[:, wi, :], in0=V[:, row, :], scalar=1.0, in1=V[:, 0, :], op0=ALU.add, op1=ALU.mult),
            reads=[r_V], writes=[r_gs])

    pi = 0
    for m in range(KC):
        ws, wb = WS[m % 2], WBF[m % 2]
        rws, rwb = r_WS[m % 2], r_WBF[m % 2]
        s.dma(lambda e, ws=ws, m=m: e.dma_start(out=ws[:].rearrange("p k n -> p (k n)"), in_=wout[m]), writes=[rws])
        if m == 0:
            for kc in range(KC):
                s.dma(lambda e, kc=kc: e.dma_start(out=X[:, kc, :], in_=xv[:, kc, :]), writes=[r_Xm[kc]])
        s.op('gpsimd', lambda e, ws=ws, wb=wb: e.tensor_copy(out=wb[:], in_=ws[:]), reads=[rws], writes=[rwb])
        for (lo, hi, which) in BLOCKS:
            w = hi - lo
            ps = PS[pi % 4]
            rp = r_PS[pi % 4]
            pi += 1
            for kc in range(KC):
                s.op('tensor', lambda e, ps=ps, wb=wb, kc=kc, lo=lo, hi=hi, w=w: e.matmul(
                    ps[:, :w], lhsT=wb[:, kc, :], rhs=Z[:, kc, lo:hi], start=(kc == 0), stop=(kc == KC - 1)),
                    reads=[rwb, r_Z], writes=[rp])
            g1row = 5 if which == 'l' else 6
            s.op('vector', lambda e, ps=ps, m=m, lo=lo, hi=hi, w=w, g1row=g1row: e.scalar_tensor_tensor(
                out=X[:, m, lo:hi], in0=ps[:, :w], scalar=V[:, g1row, m:m + 1], in1=X[:, m, lo:hi],
                op0=ALU.mult, op1=ALU.add), reads=[rp, r_V, r_Xm[m]], writes=[r_Xm[m]])

    xov = xo.rearrange("(kc p) n -> p kc n", p=128)
    for m in range(KC):
        s.dma(lambda e, m=m: e.dma_start(out=xov[:, m, :], in_=X[:, m, :]), reads=[r_Xm[m]], final=True)
    h2v = h2.rearrange("(kc p) n -> p kc n", p=128)
    ti = 0
    for (lo, hi, which) in BLOCKS:
        w = hi - lo
        emit_rstd(c, s, X, r_Xm, ones, r_ones, sqs, r_sq, S, r_S, rstd, r_rstd, lo, hi)
        wi = 0 if which == 'l' else 1
        shrow = 1 if which == 'l' else 3
        for kc in range(KC):
            tmp = tmps[ti % 2]
            rt = r_tmp[ti % 2]
            ti += 1
            s.op('vector', lambda e, tmp=tmp, kc=kc, wi=wi, lo=lo, hi=hi, w=w: e.scalar_tensor_tensor(
                out=tmp[:, :w], in0=X[:, kc, lo:hi], scalar=gs[:, wi, kc:kc + 1], in1=rstd[:, :w],
                op0=ALU.mult, op1=ALU.mult), reads=[r_Xm[kc], r_gs, r_rstd], writes=[rt])
            s.op('scalar', lambda e, tmp=tmp, kc=kc, shrow=shrow, w=w: e.activation(
                out=H2F[:, kc, :w], in_=tmp[:, :w], func=AF.Identity, bias=V[:, shrow, kc:kc + 1], scale=1.0),
                reads=[rt, r_V], writes=[r_H2F])
            s.op('scalar', lambda e, tmp=tmp, kc=kc, shrow=shrow, w=w: e.activation(
                out=H2B[:, kc, :w], in_=tmp[:, :w], func=AF.Identity, bias=V[:, shrow, kc:kc + 1], scale=1.0),
                reads=[rt, r_V], writes=[r_H2B])
            s.op('tensor', lambda e, kc=kc, w=w: e.matmul(
                L[:NE, :w], lhsT=WR[:, kc, :], rhs=H2F[:, kc, :w], start=(kc == 0), stop=(kc == KC - 1)),
                reads=[r_WR, r_H2F], writes=[r_L])
        s.dma([lambda e, q=q, lo=lo, hi=hi, w=w: e.dma_start(out=h2v[:, 4 * q:4 * q + 4, lo:hi], in_=H2B[:, 4 * q:4 * q + 4, :w])
               for q in range(4)], reads=[r_H2B], final=True)
        s.op('tensor', lambda e, w=w: e.matmul(L[:NE, :w], lhsT=zerob[:, :NE], rhs=sqs[0][:, :w], start=False, stop=True),
             reads=[r_sq[0]], writes=[r_L])
        s.op('scalar', lambda e, w=w: e.activation(out=E[:, :w], in_=L[:NE, :w], func=AF.Exp), reads=[r_L], writes=[r_E])
        s.op('tensor', lambda e, w=w: e.matmul(SM[:NE, :w], lhsT=ones[:NE, :NE], rhs=E[:, :w], start=True, stop=False),
             reads=[r_E, r_ones], writes=[r_SM])
        s.op('tensor', lambda e, w=w: e.matmul(SM[:NE, :w], lhsT=zerob[:, :NE], rhs=sqs[0][:, :w], start=False, stop=True),
             reads=[r_sq[0]], writes=[r_SM])
        s.op('vector', lambda e, w=w: e.reciprocal(out=RS[:, :w], in_=SM[:NE, :w]), reads=[r_SM], writes=[r_RS])
        s.op('vector', lambda e, w=w, lo=lo, hi=hi: e.tensor_tensor(out=AFF[:, lo:hi], in0=E[:, :w], in1=RS[:, :w], op=ALU.mult),
             reads=[r_E, r_RS], writes=[r_AFF])
    s.dma(lambda e: e.dma_start(out=aff, in_=AFF[:]), reads=[r_AFF], final=True)
    return c.finish()


NIT = 26
BIG = float(1 << 20)


def build_d():
    c = Ctx()
    s = c.s
    al_d = c.dram_in("al", [128, 128], F32)
    ac_d = c.dram_in("ac", [128, 128], F32)
    h2tok = c.dram_in("h2tok", [NTOK, D], BF16)
    wg_d = c.dram_in("wg", [2, 8, 128, KC * 128], F32)
    wu_d = c.dram_in("wu", [2, 8, 128, KC * 128], F32)
    wd_d = c.dram_in("wd", [2, 4, 128, 8 * 512], F32)
    identb_d = c.dram_in("identb", [128, 128], BF16)
    tri_d = c.dram_in("tri", [128, 128], BF16)
    blk_d = c.dram_in("blk", [128, 128], BF16)
    iota_d = c.dram_in("iota", [128, 1024], F32)
    sel_d = c.dram_in("sel", [128, 2], BF16)
    ebm_d = c.dram_in("ebm", [128, 2], F32)
    y = c.dram_out("y", [2, SLOTS, D], BF16)
    ridx = c.dram_out("ridx", [128, 2, 128], I32)

    WG = c.sb([128, KC, FF], BF16)
    WU = c.sb([128, KC, FF], BF16)
    WD = c.sb([128, 8, D], BF16)
    STG = [c.sb([128, 2048], F32) for _ in range(3)]
    XS = c.sb([128, KC, SLOTS], BF16)
    UT = c.sb([128, 8, SLOTS], BF16)
    XG = [c.sb([128, D], BF16) for _ in range(2)]
    YT = [c.sb([128, 512], BF16) for _ in range(4)]
    SGT = [c.sb([128, 512], F32) for _ in range(2)]
    IOTA = c.sb([128, 1024], F32)
    IND = [c.sb([128, 1024], BF16) for _ in range(2)]
    identb = c.sb([128, 128], BF16)
    TRI = c.sb([128, 128], BF16)
    BLK = c.sb([128, 128], BF16)
    SEL = c.sb([128, 2], BF16)
    SELH = c.sb([128, 2], BF16)
    NB = c.sb([128, 128], F32)
    INDA = [c.sb([128, 1024], BF16) for _ in range(2)]
    r_NB = Res()
    r_INDA = [Res(), Res()]
    EBM = c.sb([128, 2], F32)
    ONES = c.sb([128, 128], F32)
    A = [c.sb([128, 128], F32) for _ in range(2)]
    M_ = c.sb([128, 128], F32)
    CS = c.sb([128, 128], F32)
    JUNK = c.sb([128, 128], F32)
    RIF = c.sb([128, 128], F32)
    RII = c.sb([128, 2, 128], I32)
    mid = c.sb([128, 1], F32)
    cntp = c.sb([128, 1], BF16)
    tt = c.sb([128, 1], F32)
    tot = c.sb([128, 1], BF16)
    IDXF = c.sb([128, 9, 2], F32)
    IDXI = c.sb([128, 9, 2], I32)
    PS = [c.ps([128, 512]) for _ in range(8)]
    r_PS = [Res() for _ in range(8)]

    (r_A0, r_A1, r_id, r_tri, r_blk, r_iota, r_sel, r_ebm, r_ones, r_M, r_CS, r_junk, r_RIF, r_RII, r_mid, r_cntp,
     r_tt, r_tot, r_IDXF, r_IDXI, r_WG, r_WU, r_WD, r_XS, r_UT) = (Res() for _ in range(25))
    r_A = [r_A0, r_A1]
    r_STG = [Res() for _ in range(3)]
    r_XG = [Res(), Res()]
    r_YT = [Res() for _ in range(4)]
    r_WGm = [Res() for _ in range(8)]
    r_WUm = [Res() for _ in range(8)]
    r_WDn = [Res() for _ in range(4)]
    r_SGT = [Res(), Res()]
    r_IND = [Res(), Res()]

    for (t, d_, r) in ((A[0], al_d, r_A0), (A[1], ac_d, r_A1), (identb, identb_d, r_id), (TRI, tri_d, r_tri),
                       (BLK, blk_d, r_blk), (IOTA, iota_d, r_iota), (SEL, sel_d, r_sel), (EBM, ebm_d, r_ebm)):
        s.dma(lambda e, t=t, d_=d_: e.dma_start(out=t[:], in_=d_), writes=[r])
    s.op('vector', lambda e: e.memset(ONES[:], 1.0), writes=[r_ones])
    s.op('vector', lambda e: e.tensor_scalar(out=SELH[:], in0=SEL[:], scalar1=0.5, scalar2=None, op0=ALU.mult),
         reads=[r_sel], writes=[r_sel])

    stn = [0]

    def stage_cast(src_ap, dst_ap, rdst):
        st = STG[stn[0] % 3]
        rs_ = r_STG[stn[0] % 3]
        stn[0] += 1
        s.dma(lambda e, st=st, src_ap=src_ap: e.dma_start(out=st[:], in_=src_ap), writes=[rs_])
        return st, rs_

    def load_gu(el, m):
        for (Wt, rWm, src) in ((WG, r_WGm, wg_d), (WU, r_WUm, wu_d)):
            st, rs_ = stage_cast(src[el, m], None, None)
            s.op('scalar', lambda e, st=st, Wt=Wt, m=m: e.activation(
                out=Wt[:, :, m * 128:(m + 1) * 128], in_=st[:].rearrange("p (k n) -> p k n", k=KC), func=AF.Copy),
                reads=[rs_], writes=[rWm[m]])

    def load_d(el, nb):
        for hf in range(2):
            st, rs_ = stage_cast(wd_d[el, nb][:, hf * 2048:(hf + 1) * 2048], None, None)
            s.op('scalar', lambda e, st=st, nb=nb, hf=hf: e.activation(
                out=WD[:, hf * 4:hf * 4 + 4, nb * 512:(nb + 1) * 512], in_=st[:].rearrange("p (m n) -> p m n", m=4),
                func=AF.Copy), reads=[rs_], writes=[r_WDn[nb]])

    def load_weights(el):
        for m in range(8):
            load_gu(el, m)
        for nb in range(4):
            load_d(el, nb)

    wsteps = [(lambda m=m: load_gu(0, m)) for m in range(8)] + [(lambda nb=nb: load_d(0, nb)) for nb in range(4)]
    for _ in range(4):
        wsteps.pop(0)()

    mids = [mid, c.sb([128, 1], F32)]
    cntps = [cntp, c.sb([128, 1], BF16)]
    tts = [tt, c.sb([128, 1], F32)]
    junks = [JUNK, c.sb([128, 128], F32)]
    r_mids = [r_mid, Res()]
    r_cntps = [r_cntp, Res()]
    r_tts = [r_tt, Res()]
    r_junks = [r_junk, Res()]
    caps = [CAPL, CAPC]
    bps = [0, 2]
    for si in range(2):
        s.op('vector', lambda e, si=si: e.memset(mids[si][:], 0.5), writes=[r_mids[si]])
    for n in range(NIT):
        wn = 2.0 ** -(n + 1)
        for si in range(2):
            s.op('vector', lambda e, si=si: e.tensor_scalar(out=junks[si][:], in0=A[si][:], scalar1=mids[si][:, 0:1], scalar2=0.0,
                                                           op0=ALU.is_ge, op1=ALU.add, accum_out=cntps[si][:, 0:1]),
                 reads=[r_A[si], r_mids[si]], writes=[r_junks[si], r_cntps[si]])
        for si in range(2):
            s.op('tensor', lambda e, si=si: e.matmul(PS[bps[si]][:, 0:1], lhsT=BLK[:], rhs=cntps[si][:, 0:1], start=True, stop=True),
                 reads=[r_blk, r_cntps[si]], writes=[r_PS[bps[si]]])
        for si in range(2):
            s.op('vector', lambda e, si=si: e.tensor_scalar(out=tts[si][:], in0=PS[bps[si]][:, 0:1], scalar1=float(caps[si]), scalar2=0.5,
                                                           op0=ALU.is_ge, op1=ALU.subtract),
                 reads=[r_PS[bps[si]]], writes=[r_tts[si]])
            s.op('vector', lambda e, si=si, wn=wn: e.scalar_tensor_tensor(out=mids[si][:], in0=tts[si][:], scalar=wn, in1=mids[si][:],
                                                                         op0=ALU.mult, op1=ALU.add),
                 reads=[r_tts[si], r_mids[si]], writes=[r_mids[si]])

    def select(si, cap, nch, mslots):
        At = A[si]
        rA = r_A[si]
        mid = mids[si]
        r_mid = r_mids[si]
        wl = 2.0 ** -(NIT + 1)
        s.op('vector', lambda e: e.tensor_scalar(out=mid[:], in0=mid[:], scalar1=-wl, scalar2=None, op0=ALU.add),
             reads=[r_mid], writes=[r_mid])
        s.op('vector', lambda e: e.tensor_scalar(out=M_[:], in0=At[:], scalar1=mid[:, 0:1], scalar2=None, op0=ALU.is_ge),
             reads=[rA, r_mid], writes=[r_M])
        s.op('vector', lambda e: e.tensor_tensor_scan(out=CS[:], data0=ONES[:], data1=M_[:], initial=0.0,
                                                      op0=ALU.mult, op1=ALU.add),
             reads=[r_M, r_ones], writes=[r_CS])
        s.op('vector', lambda e: e.tensor_copy(out=tot[:], in_=CS[:, 127:128]), reads=[r_CS], writes=[r_tot])
        s.op('tensor', lambda e: e.matmul(PS[1][:, 0:1], lhsT=TRI[:], rhs=tot[:, 0:1], start=True, stop=True),
             reads=[r_tri, r_tot], writes=[r_PS[1]])
        s.op('vector', lambda e: e.tensor_scalar(out=CS[:], in0=CS[:], scalar1=PS[1][:, 0:1], scalar2=None, op0=ALU.add),
             reads=[r_PS[1], r_CS], writes=[r_CS])
        s.op('vector', lambda e: e.tensor_scalar(out=RIF[:], in0=CS[:], scalar1=EBM[:, si:si + 1], scalar2=None, op0=ALU.add),
             reads=[r_CS, r_ebm], writes=[r_RIF])
        s.op('vector', lambda e: e.tensor_tensor(out=RIF[:], in0=RIF[:], in1=M_[:], op=ALU.mult),
             reads=[r_RIF, r_M], writes=[r_RIF])
        s.op('vector', lambda e: e.tensor_scalar(out=RII[:, si, :], in0=RIF[:], scalar1=BIG, scalar2=None, op0=ALU.add),
             reads=[r_RIF], writes=[r_RII])
        use_act = (si == 0)
        if use_act:
            s.op('vector', lambda e: e.tensor_scalar(out=NB[:], in0=CS[:], scalar1=-1.0, scalar2=0.5, op0=ALU.mult, op1=ALU.add),
                 reads=[r_CS], writes=[r_NB])
        for j in range(128):
            if use_act and j % 16 == 8 and wsteps:
                wsteps.pop(0)()
            on_act = use_act and (j % 2 == 1)
            ind = INDA[(j // 2) % 2] if on_act else IND[(j // 2) % 2 if use_act else j % 2]
            rind = r_INDA[(j // 2) % 2] if on_act else r_IND[(j // 2) % 2 if use_act else j % 2]
            if on_act:
                s.op('scalar', lambda e, ind=ind, j=j: e.activation(
                    out=ind[:, :nch * mslots], in_=IOTA[:, :nch * mslots], func=AF.Sign, bias=NB[:, j:j + 1], scale=1.0),
                    reads=[r_iota, r_NB], writes=[rind])
            else:
                s.op('vector', lambda e, ind=ind, j=j: e.tensor_scalar(
                    out=ind[:, :nch * mslots], in0=IOTA[:, :nch * mslots], scalar1=CS[:, j:j + 1], scalar2=None, op0=ALU.is_ge),
                    reads=[r_iota, r_CS], writes=[rind])
            selm = SELH if on_act else SEL
            for sc in range(nch):
                s.op('tensor', lambda e, ind=ind, sc=sc, j=j, selm=selm: e.matmul(
                    PS[sc][:mslots, 0:2], lhsT=ind[:, sc * mslots:(sc + 1) * mslots], rhs=selm[:],
                    start=(j == 0), stop=(j == 127)), reads=[rind, r_sel], writes=[r_PS[sc]])
        for sc in range(nch):
            dst = sc if si == 0 else 8
            off = float(SEQ // 4) if si == 0 else float(SEQ)
            s.op('vector', lambda e, sc=sc, dst=dst, off=off: e.tensor_scalar(
                out=IDXF[:mslots, dst, :], in0=PS[sc][:mslots, 0:2], scalar1=off, scalar2=None, op0=ALU.add),
                reads=[r_PS[sc]], writes=[r_IDXF])

    select(1, CAPC, 1, 32)
    select(0, CAPL, 8, 128)
    while wsteps:
        wsteps.pop(0)()
    s.op('vector', lambda e: e.tensor_copy(out=IDXI[:], in_=IDXF[:]), reads=[r_IDXF], writes=[r_IDXI])
    s.dma(lambda e: e.dma_start(out=ridx, in_=RII[:]), reads=[r_RII], final=True)

    SBLK = [(0, 512), (512, 1024), (1024, 1056)]
    psn = [0]
    xgn = [0]
    ytn = [0]
    sgn = [0]

    def nextps():
        i = psn[0] % 8
        psn[0] += 1
        return PS[i], r_PS[i]

    def expert(el):
        for sc in range(9):
            P = 128 if sc < 8 else CAPC
            xg = XG[xgn[0] % 2]
            rxg = r_XG[xgn[0] % 2]
            xgn[0] += 1
            s.dma(lambda e, xg=xg, sc=sc, P=P: e.indirect_dma_start(
                out=xg[:P, :], out_offset=None, in_=h2tok,
                in_offset=bass.IndirectOffsetOnAxis(ap=IDXI[:P, sc, el:el + 1], axis=0),
                bounds_check=c.breg(e, NTOK - 1), oob_is_err=False), reads=[r_IDXI], writes=[rxg], eng='gpsimd')
            for hf in range(2):
                ps, rp = nextps()
                psb = ps[:].bitcast(BF16)
                for q in range(8):
                    fc = hf * 8 + q
                    s.op('tensor', lambda e, psb=psb, xg=xg, fc=fc, q=q, P=P: e.transpose(
                        out=psb[:, q * 128:q * 128 + P], in_=xg[:P, fc * 128:(fc + 1) * 128], identity=identb[:P, :P]),
                        reads=[rxg, r_id], writes=[rp])
                s.op('vector', lambda e, psb=psb, hf=hf, sc=sc, P=P: e.tensor_copy(
                    out=XS[:, hf * 8:hf * 8 + 8, sc * 128:sc * 128 + P],
                    in_=psb.rearrange("p (a b) -> p a b", a=8)[:, :, :P]), reads=[rp], writes=[r_XS])
        for m in range(8):
            for (lo, hi) in SBLK:
                w = hi - lo
                pg, rpg = nextps()
                pu, rpu = nextps()
                for kc in range(KC):
                    s.op('tensor', lambda e, pg=pg, m=m, kc=kc, lo=lo, hi=hi, w=w: e.matmul(
                        pg[:, :w], lhsT=WG[:, kc, m * 128:(m + 1) * 128], rhs=XS[:, kc, lo:hi],
                        start=(kc == 0), stop=(kc == KC - 1)), reads=[r_WGm[m], r_XS], writes=[rpg])
                for kc in range(KC):
                    s.op('tensor', lambda e, pu=pu, m=m, kc=kc, lo=lo, hi=hi, w=w: e.matmul(
                        pu[:, :w], lhsT=WU[:, kc, m * 128:(m + 1) * 128], rhs=XS[:, kc, lo:hi],
                        start=(kc == 0), stop=(kc == KC - 1)), reads=[r_WUm[m], r_XS], writes=[rpu])
                sg = SGT[sgn[0] % 2]
                rsg = r_SGT[sgn[0] % 2]
                sgn[0] += 1
                s.op('scalar', lambda e, sg=sg, pg=pg, w=w: e.activation(out=sg[:, :w], in_=pg[:, :w], func=AF.Silu),
                     reads=[rpg], writes=[rsg])
                s.op('vector', lambda e, sg=sg, pu=pu, m=m, lo=lo, hi=hi, w=w: e.tensor_tensor(
                    out=UT[:, m, lo:hi], in0=sg[:, :w], in1=pu[:, :w], op=ALU.mult), reads=[rsg, rpu], writes=[r_UT])
            if el == 0:
                load_gu(1, m)
        for nb in range(4):
            for sc in range(9):
                P = 128 if sc < 8 else CAPC
                yt = YT[ytn[0] % 4]
                ryt = r_YT[ytn[0] % 4]
                ytn[0] += 1
                py, rpy = nextps()
                for m in range(8):
                    s.op('tensor', lambda e, py=py, m=m, sc=sc, nb=nb, P=P: e.matmul(
                        py[:P, :], lhsT=UT[:, m, sc * 128:sc * 128 + P], rhs=WD[:, m, nb * 512:(nb + 1) * 512],
                        start=(m == 0), stop=(m == 7)), reads=[r_UT, r_WDn[nb]], writes=[rpy])
                s.op('scalar', lambda e, py=py, yt=yt, P=P: e.activation(
                    out=yt[:P, :], in_=py[:P, :], func=AF.Copy), reads=[rpy], writes=[ryt])
                s.dma(lambda e, yt=yt, sc=sc, nb=nb, P=P: e.dma_start(
                    out=y[el, sc * 128:sc * 128 + P, nb * 512:(nb + 1) * 512], in_=yt[:P, :]),
                    reads=[ryt], final=True, eng='scalar')
            if el == 0:
                load_d(1, nb)

    expert(0)
    expert(1)
    return c.finish()


def d_consts(k):
    p = np.arange(128)
    same = (p[:, None] // 64) == (p[None, :] // 64)
    tri = (same & (p[:, None] < p[None, :])).astype(np.float32).astype(NPBF)
    blk = same.astype(np.float32).astype(NPBF)
    iota = np.ascontiguousarray(np.broadcast_to(np.arange(1024, dtype=np.float32), (128, 1024)))
    sel = np.stack([(p // 64 == 0), (p // 64 == 1)], axis=1).astype(NPBF)
    ebm = np.zeros((128, 2), np.float32)
    e_glob = 2 * k + p // 64
    ebm[:, 0] = e_glob * SLOTS - 1 - BIG
    ebm[:, 1] = e_glob * SLOTS + CAPL - 1 - BIG
    return {"identb": np.eye(128, dtype=np.float32).astype(NPBF), "tri": tri, "blk": blk, "iota": iota,
            "sel": sel, "ebm": ebm}


def moe_inputs(inp, layer, cres):
    aff_l = np.concatenate([r["aff"][:, :TL] for r in cres], axis=1)
    aff_c = np.concatenate([r["aff"][:, TL:] for r in cres], axis=1)
    h2tok = np.ascontiguousarray(np.concatenate(
        [r["h2"][:, :TL].T for r in cres] + [r["h2"][:, TL:].T for r in cres], axis=0))
    maps = []
    for k in range(NCORES):
        al = np.ascontiguousarray(aff_l[2 * k:2 * k + 2].reshape(128, 128))
        ac = np.full((128, 128), -1.0, np.float32)
        for el in range(2):
            ac[el * 64:el * 64 + 2] = aff_c[2 * k + el].reshape(2, 128)
        def tile_gu(w):
            return np.ascontiguousarray(w.reshape(2, KC, 128, 8, 128).transpose(0, 3, 2, 1, 4).reshape(2, 8, 128, KC * 128))

        def tile_d(w):
            return np.ascontiguousarray(w.reshape(2, 8, 128, 4, 512).transpose(0, 3, 2, 1, 4).reshape(2, 4, 128, 8 * 512))
        m = {"al": al, "ac": ac, "h2tok": h2tok,
             "wg": tile_gu(inp['moe_w_gate'][layer, 2 * k:2 * k + 2]), "wu": tile_gu(inp['moe_w_up'][layer, 2 * k:2 * k + 2]),
             "wd": tile_d(inp['moe_w_down'][layer, 2 * k:2 * k + 2])}
        m.update(d_consts(k))
        maps.append(m)
    return maps, aff_l, aff_c


def combine_inputs(dres, aff_l, aff_c):
    yall = np.ascontiguousarray(np.concatenate([r["y"].reshape(2 * SLOTS, D) for r in dres], axis=0))
    ridx_l = np.concatenate([r["ridx"][:, 0, :].reshape(2, SEQ) for r in dres], axis=0)
    ridx_c = np.concatenate([r["ridx"][:, 1, :].reshape(2, 64, 128)[:, :2].reshape(2, CTX) for r in dres], axis=0)
    out = []
    for k in range(NCORES):
        ri = np.full((128, NTILE, NE), int(BIG), np.int32)
        af = np.zeros((128, NTILE, NE), np.float32)
        ri[:, :8, :] = ridx_l[:, k * TL:(k + 1) * TL].T.reshape(8, 128, NE).transpose(1, 0, 2)
        af[:, :8, :] = aff_l[:, k * TL:(k + 1) * TL].T.reshape(8, 128, NE).transpose(1, 0, 2)
        ri[:TC, 8, :] = ridx_c[:, k * TC:(k + 1) * TC].T
        af[:TC, 8, :] = aff_c[:, k * TC:(k + 1) * TC].T
        out.append((np.ascontiguousarray(ri), np.ascontiguousarray(af)))
    return yall, out


GRID_W = 64
ROWS = SEQ // GRID_W
NEG = -30000.0


def att_pattern(r):
    return 0 if r == 0 else 1 if r == 2 else 3 if r == 124 else 4 if r == 126 else 2


def build_att():
    c = Ctx()
    s = c.s
    h = c.dram_in("h", [D, NTOK], BF16)
    wqkv = c.dram_in("wqkv", [D, 768], F32)
    bt_d = c.dram_in("bt", [128, 2, 5, 5, 128], BF16)
    identb_d = c.dram_in("identb", [128, 128], BF16)
    z = c.dram_out("z", [256, NTOK], BF16)

    QT = c.sb([128, 2, NTOK], BF16)
    KT = c.sb([128, 2, NTOK], BF16)
    VT = c.sb([128, NTOK // 128, 256], BF16)
    WB = c.sb([128, KC, 768], BF16)
    STG = [c.sb([128, 2, 768], F32) for _ in range(2)]
    HT = [c.sb([128, KC, 512], BF16) for _ in range(2)]
    BT = c.sb([128, 2, 5, 5, 128], BF16)
    identb = c.sb([128, 128], BF16)
    onesb = c.sb([128, 128], BF16)
    PT = [c.sb([128, 896], BF16) for _ in range(2)]
    RD = [c.sb([128, 128], F32) for _ in range(2)]
    ZS = [c.sb([128, 2, 512], BF16) for _ in range(2)]
    PS = [c.ps([128, 512]) for _ in range(8)]
    r_PS = [Res() for _ in range(8)]
    r_QT, r_KT, r_VT, r_WB, r_BT, r_id, r_ones = (Res() for _ in range(7))
    r_STG = [Res(), Res()]
    r_HT = [Res(), Res()]
    r_PT = [Res(), Res()]
    r_RD = [Res(), Res()]
    r_ZS = [Res(), Res()]

    s.dma(lambda e: e.dma_start(out=BT[:], in_=bt_d), writes=[r_BT])
    s.dma(lambda e: e.dma_start(out=identb[:], in_=identb_d), writes=[r_id])
    s.op('vector', lambda e: e.memset(onesb[:], 1.0), writes=[r_ones])
    wv = wqkv.rearrange("(kc p) n -> p kc n", p=128)
    r_WBq = [Res() for _ in range(8)]
    for q in range(8):
        st = STG[q % 2]
        rs_ = r_STG[q % 2]
        s.dma(lambda e, st=st, q=q: e.dma_start(out=st[:], in_=wv[:, 2 * q:2 * q + 2, :]), writes=[rs_])
        s.op('scalar', lambda e, st=st, q=q: e.activation(out=WB[:, 2 * q:2 * q + 2, :], in_=st[:], func=AF.Copy),
             reads=[rs_], writes=[r_WBq[q]])

    hv = h.rearrange("(kc p) n -> p kc n", p=128)
    pi = 0
    scale = float(128 ** -0.5)
    for bi, (c0, c1) in enumerate(TBLK):
        w = c1 - c0
        ht = HT[bi % 2]
        rht = r_HT[bi % 2]
        s.dma([lambda e, ht=ht, c0=c0, c1=c1, w=w, q=q: e.dma_start(out=ht[:, 4 * q:4 * q + 4, :w], in_=hv[:, 4 * q:4 * q + 4, c0:c1])
               for q in range(4)], writes=[rht])
        for m in range(4):
            ps, rp = PS[pi % 8], r_PS[pi % 8]
            pi += 1
            for kc in range(KC):
                s.op('tensor', lambda e, ps=ps, ht=ht, m=m, kc=kc, w=w: e.matmul(
                    ps[:, :w], lhsT=WB[:, kc, m * 128:(m + 1) * 128], rhs=ht[:, kc, :w],
                    start=(kc == 0), stop=(kc == KC - 1)), reads=[r_WBq[kc // 2], rht], writes=[rp])
            if m < 2:
                s.op('scalar', lambda e, ps=ps, m=m, c0=c0, c1=c1, w=w: e.activation(
                    out=QT[:, m, c0:c1], in_=ps[:, :w], func=AF.Copy, scale=scale), reads=[rp], writes=[r_QT])
            else:
                s.op('vector', lambda e, ps=ps, m=m, c0=c0, c1=c1, w=w: e.tensor_copy(
                    out=KT[:, m - 2, c0:c1], in_=ps[:, :w]), reads=[rp], writes=[r_KT])
        for t in range(w // 128):
            ps, rp = PS[pi % 8], r_PS[pi % 8]
            pi += 1
            ti = c0 // 128 + t
            for kc in range(KC):
                s.op('tensor', lambda e, ps=ps, ht=ht, t=t, kc=kc: e.matmul(
                    ps[:, :256], lhsT=ht[:, kc, t * 128:(t + 1) * 128], rhs=WB[:, kc, 512:768],
                    start=(kc == 0), stop=(kc == KC - 1)), reads=[r_WBq[kc // 2], rht], writes=[rp])
            if t % 2 == 0:
                s.op('scalar', lambda e, ps=ps, ti=ti: e.activation(out=VT[:, ti, :], in_=ps[:, :256], func=AF.Copy),
                     reads=[rp], writes=[r_VT])
            else:
                s.op('vector', lambda e, ps=ps, ti=ti: e.tensor_copy(out=VT[:, ti, :], in_=ps[:, :256]),
                     reads=[rp], writes=[r_VT])

    un = [0]

    pend = []

    def unit(qc0, local, zs, rzs, zoff):
        for hh in range(2):
            u = un[0]
            un[0] += 1
            pend.append(unit_qk(u, hh, qc0, local, zs, rzs, zoff))
            if len(pend) > 1:
                unit_pv(*pend.pop(0))

    def flush_units():
        while pend:
            unit_pv(*pend.pop(0))

    def unit_qk(u, hh, qc0, local, zs, rzs, zoff):
        if True:
            sa, rsa = PS[(u % 2) * 4 + 0], r_PS[(u % 2) * 4 + 0]
            sb_, rsb = PS[(u % 2) * 4 + 1], r_PS[(u % 2) * 4 + 1]
            po, rpo = PS[(u % 2) * 4 + 2], r_PS[(u % 2) * 4 + 2]
            pd, rpd = PS[(u % 2) * 4 + 3], r_PS[(u % 2) * 4 + 3]
            pt, rpt = PT[u % 2], r_PT[u % 2]
            rd, rrd = RD[u % 2], r_RD[u % 2]
            chunks = []
            if local is not None:
                kc0, pat = local
                for j in range(5):
                    tgt = (sa[:, j * 128:(j + 1) * 128], rsa) if j < 4 else (sb_[:, 0:128], rsb)
                    chunks.append((tgt[0], tgt[1], kc0 + j * 128, (kc0 + j * 128) // 128, (pat, j)))
                chunks.append((sb_[:, 128:256], rsb, 0, 0, None))
                chunks.append((sb_[:, 256:384], rsb, 128, 1, None))
                nA, nB = 512, 384
            else:
                chunks.append((sa[:, 0:128], rsa, 0, 0, None))
                chunks.append((sa[:, 128:256], rsa, 128, 1, None))
                nA, nB = 256, 0
            for (ap, rr, kcol, vt, bias) in chunks:
                s.op('tensor', lambda e, ap=ap, kcol=kcol, hh=hh, bias=bias: e.matmul(
                    ap, lhsT=KT[:, hh, kcol:kcol + 128], rhs=QT[:, hh, qc0:qc0 + 128], start=True, stop=(bias is None)),
                    reads=[r_KT, r_QT], writes=[rr])
                if bias is not None:
                    s.op('tensor', lambda e, ap=ap, hh=hh, bias=bias: e.matmul(
                        ap, lhsT=identb[:], rhs=BT[:, hh, bias[0], bias[1], :], start=False, stop=True),
                        reads=[r_id, r_BT], writes=[rr])
            s.op('scalar', lambda e, sa=sa, pt=pt, nA=nA: e.activation(out=pt[:, 0:nA], in_=sa[:, 0:nA], func=AF.Exp),
                 reads=[rsa], writes=[rpt])
            if nB:
                s.op('scalar', lambda e, sb_=sb_, pt=pt, nB=nB: e.activation(out=pt[:, 512:512 + nB], in_=sb_[:, 0:nB], func=AF.Exp),
                     reads=[rsb], writes=[rpt])
            return (hh, chunks, po, rpo, pd, rpd, pt, rpt, rd, rrd, zs, rzs, zoff)

    def unit_pv(hh, chunks, po, rpo, pd, rpd, pt, rpt, rd, rrd, zs, rzs, zoff):
        if True:
            nchk = len(chunks)
            for ci, (ap, rr, kcol, vt, bias) in enumerate(chunks):
                pcol = ci * 128 if ci < 4 else 512 + (ci - 4) * 128
                s.op('tensor', lambda e, po=po, vt=vt, hh=hh, pt=pt, pcol=pcol, ci=ci, nchk=nchk: e.matmul(
                    po[:, 0:128], lhsT=VT[:, vt, hh * 128:(hh + 1) * 128], rhs=pt[:, pcol:pcol + 128],
                    start=(ci == 0), stop=(ci == nchk - 1)), reads=[r_VT, rpt], writes=[rpo])
            for ci in range(nchk):
                pcol = ci * 128 if ci < 4 else 512 + (ci - 4) * 128
                s.op('tensor', lambda e, pd=pd, pt=pt, pcol=pcol, ci=ci, nchk=nchk: e.matmul(
                    pd[:, 0:128], lhsT=onesb[:], rhs=pt[:, pcol:pcol + 128],
                    start=(ci == 0), stop=(ci == nchk - 1)), reads=[r_ones, rpt], writes=[rpd])
            s.op('vector', lambda e, rd=rd, pd=pd: e.reciprocal(out=rd[:], in_=pd[:, 0:128]), reads=[rpd], writes=[rrd])
            s.op('vector', lambda e, rd=rd, po=po, hh=hh: e.tensor_tensor(
                out=zs[:, hh, zoff:zoff + 128], in0=po[:, 0:128], in1=rd[:], op=ALU.mult),
                reads=[rpo, rrd], writes=[rzs])

    zn = 0
    zs, rzs = ZS[zn % 2], r_ZS[zn % 2]
    zn += 1
    for qt in range(2):
        unit(qt * 128, None, zs, rzs, qt * 128)
    flush_units()
    s.dma([lambda e, zs=zs, hh=hh: e.dma_start(out=z[hh * 128:(hh + 1) * 128, 0:256], in_=zs[:, hh, 0:256]) for hh in range(2)],
          reads=[rzs], final=True)
    for rp4 in range(16):
        zs, rzs = ZS[zn % 2], r_ZS[zn % 2]
        zn += 1
        for i in range(4):
            r = 2 * (rp4 * 4 + i)
            base = min(max(r - 4, 0), 118)
            unit(CTX + r * GRID_W, (CTX + base * GRID_W, att_pattern(r)), zs, rzs, i * 128)
        flush_units()
        c0 = CTX + rp4 * 512
        s.dma([lambda e, zs=zs, hh=hh, c0=c0: e.dma_start(out=z[hh * 128:(hh + 1) * 128, c0:c0 + 512], in_=zs[:, hh, :]) for hh in range(2)],
              reads=[rzs], final=True)
    return c.finish()


def att_bias_tiles(rpb2):
    out = np.full((2, 5, 640, 128), NEG, np.float32)
    cq = np.arange(GRID_W)
    cs = np.clip(cq - 8, 0, GRID_W - 16)
    for pat, (r, base) in enumerate(((0, 0), (2, 0), (4, 0), (124, 118), (126, 118))):
        for i in range(2):
            rs = min(max(r + i - 4, 0), ROWS - 8)
            for kr in range(10):
                ar = base + kr
                if not (rs <= ar < rs + 8):
                    continue
                dr = ar - (r + i) + 7
                for c_ in range(GRID_W):
                    kcs = np.arange(cs[c_], cs[c_] + 16)
                    out[:, pat, kr * 64 + kcs, i * 64 + c_] = rpb2[:, dr, kcs - c_ + 15]
    out = out.reshape(2, 5, 5, 128, 128).transpose(3, 0, 1, 2, 4)
    return np.ascontiguousarray(out).astype(NPBF)


def att_inputs(inp, j, k, h_all):
    w = inp['attn_w_qkv'][j]
    wqkv = np.ascontiguousarray(np.concatenate(
        [w[:, i * D + k * 256:i * D + (k + 1) * 256] for i in range(3)], axis=1))
    return {"h": h_all, "wqkv": wqkv, "bt": att_bias_tiles(inp['attn_rpb'][j][2 * k:2 * k + 2]),
            "identb": np.eye(128, dtype=np.float32).astype(NPBF)}


_PROGS = {}


def _prog(name, builder):
    if name not in _PROGS:
        _PROGS[name] = builder()
    return _PROGS[name]


def kernel(**inputs):
    inp = {k: np.asarray(v) for k, v in inputs.items()}
    mods = run_ada(inp)
    x = inp['x'][0]
    ctx = inp['ctx'][0]
    XT = [np.ascontiguousarray(np.concatenate([x[k * TL:(k + 1) * TL].T, ctx[k * TC:(k + 1) * TC].T], axis=1))
          for k in range(NCORES)]
    identb = np.eye(128, dtype=np.float32).astype(NPBF)
    comb = None
    prev_g2 = None
    for i in range(DEPTH):
        j = i // 2
        ml = mods[i, 0].reshape(6, D)
        mc = mods[i, 1].reshape(6, D)
        rows = {0: inp['norm_mix_g'][i], 1: ml[0], 2: ml[1], 3: mc[0], 4: mc[1]}
        if comb is None:
            vec = pack_vec(rows)
            res = run(_prog('t1n', lambda: build_t1('norm', False)), [{"x": XT[k], "vec": vec} for k in range(NCORES)])
        else:
            rows[5], rows[6] = prev_g2
            vec = pack_vec(rows)
            yall, tiles = comb
            res = run(_prog('t1c', lambda: build_t1('norm', True)),
                      [{"x": XT[k], "vec": vec, "yall": yall, "ridx": tiles[k][0], "afft": tiles[k][1], "identb": identb}
                       for k in range(NCORES)])
            XT = [r["xo"] for r in res]
        h_all = gather_h_all([r["h"] for r in res])
        if i % 2 == 0:
            zres = run(_prog('lru', build_lru), [lru_inputs(inp, j, k, h_all) for k in range(NCORES)])
            wout = tile_wout(inp['lru_w_out'][j])
        else:
            zres = run(_prog('att', build_att), [att_inputs(inp, j, k, h_all) for k in range(NCORES)])
            wout = tile_wout(inp['attn_w_out'][j])
        zs = scatter_z([r["z"] for r in zres])
        vec2 = pack_vec({0: inp['norm_ffn_g'][i], 1: ml[3], 2: ml[4], 3: mc[3], 4: mc[4], 5: ml[2], 6: mc[2]})
        wr = np.ascontiguousarray(inp['moe_router'][i].reshape(KC, 128, NE).transpose(1, 0, 2))
        cres = run(_prog('c', build_c), [{"z": zs[k], "x": XT[k], "wout": wout, "vec": vec2, "wr": wr}
                                          for k in range(NCORES)])
        XT = [r["xo"] for r in cres]
        maps, aff_l, aff_c = moe_inputs(inp, i, cres)
        dres = run(_prog('d', build_d), maps)
        comb = combine_inputs(dres, aff_l, aff_c)
        prev_g2 = (ml[5], mc[5])
    rows = {0: inp['final_norm_g'], 5: prev_g2[0], 6: prev_g2[1]}
    vec = pack_vec(rows)
    yall, tiles = comb
    res = run(_prog('t1f', lambda: build_t1('final', True)),
              [{"x": XT[k], "vec": vec, "yall": yall, "ridx": tiles[k][0], "afft": tiles[k][1], "identb": identb}
               for k in range(NCORES)])
    out = np.concatenate([r["o"].T for r in res], axis=0)[None]
    return np.ascontiguousarray(out.astype(np.float32))
```

```python
import contextlib
import numpy as np
import ml_dtypes
import concourse.bass as bass
import concourse.mybir as mybir
from concourse.bass_utils import run_bass_kernel_spmd

F32 = mybir.dt.float32
BF16 = mybir.dt.bfloat16
I32 = mybir.dt.int32
U32 = mybir.dt.uint32
AF = mybir.ActivationFunctionType
ALU = mybir.AluOpType
NPBF = ml_dtypes.bfloat16

NCORES = 8
D = 2048
KC = 16
SEQ = 8192
CTX = 256
NTOK = SEQ + CTX
TL = SEQ // NCORES
TC = CTX // NCORES
TT = TL + TC
DEPTH = 4
NE = 16
FF = 1024
CAPL = 1024
CAPC = 32
SLOTS = CAPL + CAPC
EPS = 1e-6
ENGS = ('sync', 'scalar', 'vector', 'gpsimd', 'tensor')


class Res:
    __slots__ = ('w', 'r', 'dkey', 'name')

    def __init__(self, name=''):
        self.w = None
        self.r = []
        self.dkey = None
        self.name = name


class Sched:
    def __init__(self, nc, stack):
        self.nc = nc
        self.stack = stack
        self.q = {e: [] for e in ENGS}
        self.esem = {}
        self.cnt = {e: 0 for e in ENGS}
        self.seen = {e: {} for e in ENGS}
        self.dsems = {}
        self.dcnt = {}
        self.finals = []

    def _semof(self, key):
        if key in ENGS:
            if key not in self.esem:
                self.esem[key] = self.stack.enter_context(self.nc.semaphore("es_" + key))
            return self.esem[key]
        return self.dsems[key]

    def _deps(self, eng, reads, writes):
        toks = []
        for r in reads:
            if r.w is not None:
                toks.append(r.w)
        for w in writes:
            if w.w is not None:
                toks.append(w.w)
            toks.extend(w.r)
        waits = []
        for (key, val) in toks:
            if eng == 'tensor' and key == 'tensor':
                continue
            if self.seen[eng].get(key, 0) >= val:
                continue
            self.seen[eng][key] = val
            waits.append((self._semof(key), val))
        return waits

    def _commit(self, tok, reads, writes):
        for r in reads:
            r.r.append(tok)
        for w in writes:
            w.w = tok
            w.r = []

    def op(self, eng, fn, reads=(), writes=()):
        waits = self._deps(eng, reads, writes)
        self.cnt[eng] += 1
        tok = (eng, self.cnt[eng])
        sem = self._semof(eng)

        def emit(e):
            for s, v in waits:
                e.wait_ge(s, v)
            fn(e).then_inc(sem, 1)
        self.q[eng].append(emit)
        self._commit(tok, reads, writes)
        return tok

    def dma(self, fns, reads=(), writes=(), eng='sync', final=False):
        if callable(fns):
            fns = [fns]
        waits = self._deps(eng, reads, writes)
        own = writes[0] if writes else reads[0]
        if own.dkey is None:
            own.dkey = ('d', len(self.dsems))
            self.dsems[own.dkey] = self.stack.enter_context(
                self.nc.semaphore("ds%d" % len(self.dsems)))
            self.dcnt[own.dkey] = 0
        key = own.dkey
        sem = self.dsems[key]
        self.dcnt[key] += 16 * len(fns)
        tok = (key, self.dcnt[key])

        def emit(e):
            for s, v in waits:
                e.wait_ge(s, v)
            for f in fns:
                f(e).then_inc(sem, 16)
        self.q[eng].append(emit)
        self._commit(tok, reads, writes)
        if final:
            self.finals.append(tok)
        return tok

    def emit_all(self):
        fin = [(self._semof(k), v) for (k, v) in self.finals]
        with self.nc.Block() as block:
            for eng in ENGS:
                if not self.q[eng] and eng != 'sync':
                    continue

                def body(e, eng=eng):
                    for f in self.q[eng]:
                        f(e)
                    if eng == 'sync':
                        for s, v in fin:
                            e.wait_ge(s, v)
                getattr(block, eng)(body)


class Ctx:
    def __init__(self):
        self.nc = bass.Bass("TRN2", target_bir_lowering=False)
        self.stack = contextlib.ExitStack()
        self.s = Sched(self.nc, self.stack)
        self.n = 0

    def dram_in(self, name, shape, dt):
        return self.nc.dram_tensor(name, list(shape), dt, kind="ExternalInput").ap()

    def dram_out(self, name, shape, dt):
        return self.nc.dram_tensor(name, list(shape), dt, kind="ExternalOutput").ap()

    def sb(self, shape, dt, name=None):
        self.n += 1
        return self.stack.enter_context(
            self.nc.sbuf_tensor(name or ("t%d" % self.n), list(shape), dt))

    def ps(self, shape, dt=F32, name=None):
        self.n += 1
        return self.stack.enter_context(
            self.nc.psum_tensor(name or ("p%d" % self.n), list(shape), dt))

    def breg(self, e, val):
        if not hasattr(self, '_bregs'):
            self._bregs = {}
        if val not in self._bregs:
            self._bregs[val] = e.to_reg(val)
        return self._bregs[val]

    def finish(self):
        self.s.emit_all()
        self.stack.close()
        return self.nc


def run(nc, in_maps):
    return run_bass_kernel_spmd(nc, in_maps, core_ids=list(range(NCORES))).results


ADA_COLS = 6 * D // NCORES
ADA_J = ADA_COLS // 128


def build_ada():
    c = Ctx()
    s = c.s
    cc = c.dram_in("cc", [128, KC, 2], F32)
    w = c.dram_in("w", [DEPTH, D, ADA_COLS], F32)
    b = c.dram_in("b", [128, DEPTH * ADA_J], F32)
    o = c.dram_out("o", [128, DEPTH * ADA_J, 2], F32)
    cct = c.sb([128, KC, 2], F32)
    sg = c.sb([128, KC, 2], F32)
    st = c.sb([128, KC, 2], F32)
    bt = c.sb([128, DEPTH * ADA_J], F32)
    ot = c.sb([128, DEPTH * ADA_J, 2], F32)
    wts = [c.sb([128, KC, 384], F32) for _ in range(2)]
    pss = [c.ps([128, 512]) for _ in range(2)]
    r_cc, r_st, r_b, r_o = Res(), Res(), Res(), Res()
    zb = c.sb([128, 128], BF16)
    r_zb = Res()
    s.op('vector', lambda e: e.memset(zb[:], 0.0), writes=[r_zb])
    r_w = [Res(), Res()]
    r_p = [Res(), Res()]
    s.dma(lambda e: e.dma_start(out=cct[:], in_=cc), writes=[r_cc])
    s.dma(lambda e: e.dma_start(out=bt[:], in_=b), writes=[r_b])
    r_sg = Res()
    s.op('scalar', lambda e: e.activation(out=sg[:], in_=cct[:], func=AF.Sigmoid), reads=[r_cc], writes=[r_sg])
    s.op('vector', lambda e: e.tensor_tensor(out=st[:], in0=sg[:], in1=cct[:], op=ALU.mult),
         reads=[r_sg, r_cc], writes=[r_st])
    n = 0
    pi = 0
    for l in range(DEPTH):
        wv = w[l].rearrange("(kc p) n -> p kc n", p=128)
        for g in range(4):
            wt = wts[n % 2]
            rw = r_w[n % 2]
            n += 1
            s.dma(lambda e, wt=wt, wv=wv, g=g: e.dma_start(out=wt[:], in_=wv[:, :, g * 384:(g + 1) * 384]),
                  writes=[rw])
            for j in range(3):
                ps = pss[pi % 2]
                rp = r_p[pi % 2]
                pi += 1
                idx = l * ADA_J + g * 3 + j
                for kc in range(KC):
                    s.op('tensor', lambda e, ps=ps, wt=wt, j=j, kc=kc: e.matmul(
                        ps[:, 0:2], lhsT=wt[:, kc, j * 128:(j + 1) * 128], rhs=st[:, kc, :],
                        start=(kc == 0), stop=(kc == KC - 1)),
                        reads=[rw, r_st], writes=[rp])
                s.op('tensor', lambda e, ps=ps: e.matmul(ps[:, 0:2], lhsT=zb[:], rhs=zb[:, 0:2], start=False, stop=True),
                     reads=[r_zb], writes=[rp])
                s.op('scalar', lambda e, ps=ps, idx=idx: e.activation(
                    out=ot[:, idx, :], in_=ps[:, 0:2], func=AF.Identity, bias=bt[:, idx:idx + 1], scale=1.0),
                    reads=[rp, r_b], writes=[r_o])
    s.dma(lambda e: e.dma_start(out=o, in_=ot[:]), reads=[r_o], final=True)
    return c.finish()


def run_ada(inp):
    nc = build_ada()
    cvec = np.stack([inp['c'][0], inp['c_ctx']], axis=-1).astype(np.float32)
    cc = np.ascontiguousarray(cvec.reshape(KC, 128, 2).transpose(1, 0, 2))
    maps = []
    for k in range(NCORES):
        wk = np.ascontiguousarray(inp['ada_w'][:, :, k * ADA_COLS:(k + 1) * ADA_COLS])
        bk = inp['ada_b'][:, k * ADA_COLS:(k + 1) * ADA_COLS].reshape(DEPTH, ADA_J, 128)
        bk = np.ascontiguousarray(bk.transpose(2, 0, 1).reshape(128, DEPTH * ADA_J))
        maps.append({"cc": cc, "w": wk, "b": bk})
    res = run(nc, maps)
    mods = np.zeros((DEPTH, 2, 6 * D), np.float32)
    for k in range(NCORES):
        ok = res[k]["o"].reshape(128, DEPTH, ADA_J, 2)
        mods[:, :, k * ADA_COLS:(k + 1) * ADA_COLS] = ok.transpose(1, 3, 2, 0).reshape(DEPTH, 2, ADA_COLS)
    return mods


def fm(v):
    return np.ascontiguousarray(np.asarray(v, np.float32).reshape(KC, 128).T)


BLOCKS = [(0, 512, 'l'), (512, 1024, 'l'), (1024, 1056, 'c')]
NTILE = 9
YROWS = NE * SLOTS


def emit_rstd(c, s, X, r_X, ones, r_ones, sqs, r_sq, S, r_S, rstd, r_rstd, lo, hi):
    w = hi - lo
    for kc in range(KC):
        sq = sqs[kc % 2]
        rq = r_sq[kc % 2]
        s.op('scalar', lambda e, sq=sq, kc=kc: e.activation(out=sq[:, :w], in_=X[:, kc, lo:hi], func=AF.Square),
             reads=[r_X[kc] if isinstance(r_X, list) else r_X], writes=[rq])
        s.op('tensor', lambda e, sq=sq, kc=kc: e.matmul(S[:, :w], lhsT=c.onesb[:], rhs=sq[:, :w],
                                                       start=(kc == 0), stop=(kc == KC - 1)),
             reads=[rq, r_ones], writes=[r_S])
    s.op('scalar', lambda e: e.activation(out=rstd[:, :w], in_=S[:, :w], func=AF.Sqrt, bias=c.epsb[:, 0:1], scale=1.0 / D),
         reads=[r_S, r_ones], writes=[r_rstd])
    s.op('vector', lambda e: e.reciprocal(out=rstd[:, :w], in_=rstd[:, :w]), reads=[r_rstd], writes=[r_rstd])


def build_t1(mode, combine):
    c = Ctx()
    s = c.s
    x = c.dram_in("x", [D, TT], F32)
    vec = c.dram_in("vec", [128, 8, KC], F32)
    X = c.sb([128, KC, TT], F32)
    V = c.sb([128, 8, KC], F32)
    ones = c.sb([128, 128], F32)
    c.epsb = c.sb([128, 1], F32)
    sqs = [c.sb([128, 512], BF16) for _ in range(2)]
    c.onesb = c.sb([128, 128], BF16)
    tmps = [c.sb([128, 512], F32) for _ in range(2)]
    rstd = c.sb([128, 512], F32)
    gs = c.sb([128, 2, KC], F32)
    S = c.ps([128, 512])
    r_X, r_V, r_ones, r_S, r_rstd, r_gs = Res(), Res(), Res(), Res(), Res(), Res()
    r_sq = [Res(), Res()]
    r_tmp = [Res(), Res()]
    s.op('vector', lambda e: e.memset(ones[:], 1.0), writes=[r_ones])
    s.op('vector', lambda e: e.memset(c.epsb[:], EPS), writes=[r_ones])
    s.op('vector', lambda e: e.memset(c.onesb[:], 1.0), writes=[r_ones])
    xv = x.rearrange("(kc p) n -> p kc n", p=128)
    s.dma([lambda e, q=q: e.dma_start(out=X[:, 4 * q:4 * q + 4, :], in_=xv[:, 4 * q:4 * q + 4, :]) for q in range(4)],
          writes=[r_X])
    s.dma(lambda e: e.dma_start(out=V[:], in_=vec), writes=[r_V])
    for wi, row in ((0, 2), (1, 4)):
        s.op('vector', lambda e, wi=wi, row=row: e.scalar_tensor_tensor(
            out=gs[:, wi, :], in0=V[:, row, :], scalar=1.0, in1=V[:, 0, :], op0=ALU.add, op1=ALU.mult),
            reads=[r_V], writes=[r_gs])

    if combine:
        yall = c.dram_in("yall", [YROWS, D], BF16)
        ridx = c.dram_in("ridx", [128, NTILE, NE], I32)
        gsel = c.dram_in("afft", [128, NTILE, NE], F32)
        RI = c.sb([128, NTILE, NE], I32)
        GS = c.sb([128, NTILE, NE], F32)
        identb_d = c.dram_in("identb", [128, 128], BF16)
        identb = c.sb([128, 128], BF16)
        r_idb = Res()
        s.dma(lambda e: e.dma_start(out=identb[:], in_=identb_d), writes=[r_idb])
        r_RI, r_GS = Res(), Res()
        s.dma(lambda e: e.dma_start(out=RI[:], in_=ridx), writes=[r_RI])
        s.dma(lambda e: e.dma_start(out=GS[:], in_=gsel), writes=[r_GS])
        MSK = c.sb([128, NTILE, NE], F32)
        r_MSK = Res()
        s.op('vector', lambda e: e.tensor_scalar(out=MSK[:], in0=RI[:], scalar1=BIG / 2, scalar2=None, op0=ALU.is_lt),
             reads=[r_RI], writes=[r_MSK])
        s.op('vector', lambda e: e.tensor_tensor(out=GS[:], in0=GS[:], in1=MSK[:], op=ALU.mult),
             reads=[r_MSK, r_GS], writes=[r_GS])
        GBS = [[c.sb([128, D], BF16) for _ in range(8)] for _ in range(2)]
        DGT = [c.sb([128, 8, 128], BF16) for _ in range(2)]
        PB = [[S] + [c.ps([128, 512]) for _ in range(3)], [c.ps([128, 512]) for _ in range(4)]]
        r_GBS = [[Res() for _ in range(8)] for _ in range(2)]
        r_DGT = [Res(), Res()]
        r_PB = [[r_S] + [Res() for _ in range(3)], [Res() for _ in range(4)]]
        for st_ in range(2):
            for b8 in range(8):
                s.op('vector', lambda e, st_=st_, b8=b8: e.memset(GBS[st_][b8][:], 0.0), writes=[r_GBS[st_][b8]])
        hn = 0
        for j in range(NTILE):
            nt = 128 if j < 8 else TC
            c0 = j * 128
            g2row = 5 if j < 8 else 6
            for hf in range(2):
                st_ = hn % 2
                hn += 1
                for e8 in range(8):
                    ex = hf * 8 + e8
                    gb = GBS[st_][e8]
                    s.dma(lambda e, gb=gb, j=j, ex=ex: e.indirect_dma_start(
                        out=gb[:], out_offset=None, in_=yall,
                        in_offset=bass.IndirectOffsetOnAxis(ap=RI[:, j, ex:ex + 1], axis=0),
                        bounds_check=c.breg(e, YROWS - 1), oob_is_err=False),
                        reads=[r_RI], writes=[r_GBS[st_][e8]], eng='gpsimd')
                    s.op('vector', lambda e, st_=st_, e8=e8, j=j, ex=ex: e.tensor_scalar(
                        out=DGT[st_][:, e8, :], in0=identb[:], scalar1=GS[:, j, ex:ex + 1], scalar2=None, op0=ALU.mult),
                        reads=[r_idb, r_GS], writes=[r_DGT[st_]])
                for fc in range(KC):
                    pb, rpb = PB[st_][fc // 4], r_PB[st_][fc // 4]
                    for e8 in range(8):
                        s.op('tensor', lambda e, pb=pb, st_=st_, fc=fc, e8=e8: e.matmul(
                            pb[:, (fc % 4) * 128:(fc % 4 + 1) * 128], lhsT=GBS[st_][e8][:, fc * 128:(fc + 1) * 128],
                            rhs=DGT[st_][:, e8, :], start=(e8 == 0), stop=(e8 == 7)),
                            reads=[r_GBS[st_][e8], r_DGT[st_]], writes=[rpb])
                for fc in range(KC):
                    pb, rpb = PB[st_][fc // 4], r_PB[st_][fc // 4]
                    s.op('vector', lambda e, pb=pb, fc=fc, nt=nt, c0=c0, g2row=g2row: e.scalar_tensor_tensor(
                        out=X[:, fc, c0:c0 + nt], in0=pb[:, (fc % 4) * 128:(fc % 4) * 128 + nt],
                        scalar=V[:, g2row, fc:fc + 1], in1=X[:, fc, c0:c0 + nt], op0=ALU.mult, op1=ALU.add),
                        reads=[rpb, r_V, r_X], writes=[r_X])
        if mode == 'norm':
            xo = c.dram_out("xo", [D, TT], F32)
            xov = xo.rearrange("(kc p) n -> p kc n", p=128)
            s.dma([lambda e, q=q: e.dma_start(out=xov[:, 4 * q:4 * q + 4, :], in_=X[:, 4 * q:4 * q + 4, :])
                   for q in range(4)], reads=[r_X], final=True)

    if mode == 'norm':
        h = c.dram_out("h", [D, TT], BF16)
        H = c.sb([128, KC, TT], BF16)
        r_H = Res()
        ti = 0
        for (lo, hi, which) in BLOCKS:
            w = hi - lo
            emit_rstd(c, s, X, r_X, ones, r_ones, sqs, r_sq, S, r_S, rstd, r_rstd, lo, hi)
            wi = 0 if which == 'l' else 1
            shrow = 1 if which == 'l' else 3
            for kc in range(KC):
                tmp = tmps[ti % 2]
                rt = r_tmp[ti % 2]
                ti += 1
                s.op('vector', lambda e, tmp=tmp, kc=kc, wi=wi, lo=lo, hi=hi, w=w: e.scalar_tensor_tensor(
                    out=tmp[:, :w], in0=X[:, kc, lo:hi], scalar=gs[:, wi, kc:kc + 1], in1=rstd[:, :w],
                    op0=ALU.mult, op1=ALU.mult), reads=[r_X, r_gs, r_rstd], writes=[rt])
                s.op('scalar', lambda e, tmp=tmp, kc=kc, shrow=shrow, lo=lo, hi=hi, w=w: e.activation(
                    out=H[:, kc, lo:hi], in_=tmp[:, :w], func=AF.Identity, bias=V[:, shrow, kc:kc + 1], scale=1.0),
                    reads=[rt, r_V], writes=[r_H])
        hv = h.rearrange("(kc p) n -> p kc n", p=128)
        s.dma([lambda e, q=q: e.dma_start(out=hv[:, 4 * q:4 * q + 4, :], in_=H[:, 4 * q:4 * q + 4, :])
               for q in range(4)], reads=[r_H], final=True)
    else:
        o = c.dram_out("o", [D, TL], F32)
        OB = [c.sb([128, KC, 512], F32)]
        r_OB = [Res()]
        ov = o.rearrange("(kc p) n -> p kc n", p=128)
        for bi_, (lo, hi, which) in enumerate(BLOCKS[:2]):
            w = hi - lo
            O, r_O = OB[0], r_OB[0]
            emit_rstd(c, s, X, r_X, ones, r_ones, sqs, r_sq, S, r_S, rstd, r_rstd, lo, hi)
            for kc in range(KC):
                s.op('vector', lambda e, kc=kc, lo=lo, hi=hi, w=w, O=O: e.scalar_tensor_tensor(
                    out=O[:, kc, :w], in0=X[:, kc, lo:hi], scalar=V[:, 0, kc:kc + 1], in1=rstd[:, :w],
                    op0=ALU.mult, op1=ALU.mult), reads=[r_X, r_V, r_rstd], writes=[r_O])
            s.dma([lambda e, q=q, lo=lo, hi=hi, O=O: e.dma_start(out=ov[:, 4 * q:4 * q + 4, lo:hi], in_=O[:, 4 * q:4 * q + 4, :])
                   for q in range(4)], reads=[r_O], final=True)
    return c.finish()


def tile_wout(w):
    return np.ascontiguousarray(w.reshape(KC, 128, KC, 128).transpose(2, 1, 0, 3).reshape(KC, 128, KC * 128))


def pack_vec(rows):
    v = np.zeros((128, 8, KC), np.float32)
    for i, r in rows.items():
        v[:, i, :] = fm(r)
    return v


TBLK = [(0, 256)] + [(256 + 512 * i, 256 + 512 * (i + 1)) for i in range(16)]
RECW = NTOK + 5


def rec_col(c0):
    return c0 + (2 if c0 < 256 else 4)


def build_lru():
    c = Ctx()
    s = c.s
    h = c.dram_in("h", [D, NTOK], BF16)
    win = c.dram_in("win", [D, 512], F32)
    cw_d = c.dram_in("cw", [128, 2, 8], F32)
    gw_d = c.dram_in("gw", [2, 2, 256, 256], F32)
    gv_d = c.dram_in("gv", [128, 2, 8], F32)
    z = c.dram_out("z", [256, NTOK], BF16)

    G = c.sb([128, 2, NTOK], BF16)
    REC = c.sb([128, 2, RECW], F32)
    Hs = c.sb([128, 2, NTOK], BF16)
    STG = [c.sb([128, 2, 512], F32)] * 2
    AR = c.sb([128, 13312], F32)
    GVH = c.sb([128, 2, 4], F32)
    r_GVH = Res()
    WB = AR[:, 8192:12288].bitcast(BF16).rearrange("p (k n) -> p k n", k=KC)
    HT = [AR[:, 0:4096].bitcast(BF16).rearrange("p (k n) -> p k n", k=KC),
          AR[:, 4096:8192].bitcast(BF16).rearrange("p (k n) -> p k n", k=KC)]
    CW = c.sb([128, 2, 8], F32)
    GV = c.sb([128, 2, 8], F32)
    GWS = c.sb([128, 2, 256], F32)
    GWB = c.sb([128, 4, 2, 256], BF16)
    CF = c.sb([128, 2, 16], F32)
    onec = c.sb([128, 1], F32)
    PS = [c.ps([128, 512]) for _ in range(8)]
    r_PS = [Res() for _ in range(8)]

    r_G, r_REC, r_H, r_WB, r_CW, r_GV, r_GWB, r_CF, r_one = (Res() for _ in range(9))
    r_STG = [Res()] * 2
    r_HT = [Res(), Res()]
    r_GWS = Res()

    s.dma(lambda e: e.dma_start(out=CW[:], in_=cw_d), writes=[r_CW])
    s.dma(lambda e: e.dma_start(out=GV[:], in_=gv_d), writes=[r_GV])
    s.op('vector', lambda e: e.tensor_scalar(out=GVH[:], in0=GV[:, :, 0:4], scalar1=0.5, scalar2=None, op0=ALU.mult),
         reads=[r_GV], writes=[r_GVH])
    s.op('vector', lambda e: e.memset(onec[:], 0.25), writes=[r_one])
    for (a, b) in ((0, 2), (258, 260), (RECW - 1, RECW)):
        s.op('vector', lambda e, a=a, b=b: e.memset(REC[:, :, a:b], 0.0), writes=[r_REC])

    r_WBq = [Res() for _ in range(8)]
    wv = win.rearrange("(kc p) n -> p kc n", p=128)
    for q in range(8):
        st = STG[q % 2]
        rs_ = r_STG[q % 2]
        s.dma(lambda e, st=st, q=q: e.dma_start(out=st[:], in_=wv[:, 2 * q:2 * q + 2, :]), writes=[rs_])
        s.op('scalar', lambda e, st=st, q=q: e.activation(out=WB[:, 2 * q:2 * q + 2, :], in_=st[:], func=AF.Copy),
             reads=[rs_], writes=[r_WBq[q]])
    for dg in range(4):
        s.dma(lambda e, dg=dg: e.dma_start(out=GWS[:], in_=gw_d[dg // 2, dg % 2].rearrange("(kc p) n -> p kc n", p=128)),
              writes=[r_GWS])
        s.op('gpsimd', lambda e, dg=dg: e.tensor_copy(out=GWB[:, dg, :, :], in_=GWS[:]), reads=[r_GWS], writes=[r_GWB])

    lam = GV[:, :, 4:6]
    T = [c.sb([128, 2, 2], F32) for _ in range(6)]
    r_T = Res()
    tal, tx, tsv, ts2, tp, trl = T

    def vop(fn, reads=(), writes=()):
        return s.op('vector', fn, reads=[r_T, r_GV] + list(reads), writes=[r_T] + list(writes))
    vop(lambda e: e.tensor_scalar(out=tal[:], in0=lam, scalar1=-1.0, scalar2=None, op0=ALU.mult))
    vop(lambda e: e.tensor_tensor(out=tal[:], in0=tal[:], in1=lam, op=ALU.max))
    s.op('scalar', lambda e: e.activation(out=tx[:], in_=tal[:], func=AF.Exp, scale=-1.0), reads=[r_T], writes=[r_T])
    vop(lambda e: e.tensor_scalar(out=tsv[:], in0=tx[:], scalar1=2.0, scalar2=None, op0=ALU.add))
    vop(lambda e: e.reciprocal(out=tsv[:], in_=tsv[:]))
    vop(lambda e: e.tensor_tensor(out=tsv[:], in0=tsv[:], in1=tx[:], op=ALU.mult))
    vop(lambda e: e.tensor_tensor(out=ts2[:], in0=tsv[:], in1=tsv[:], op=ALU.mult))
    vop(lambda e: e.tensor_scalar(out=tp[:], in0=ts2[:], scalar1=1.0 / 13, scalar2=1.0 / 11, op0=ALU.mult, op1=ALU.add))
    for cf in (1.0 / 9, 1.0 / 7, 1.0 / 5, 1.0 / 3, 1.0):
        vop(lambda e: e.tensor_tensor(out=tp[:], in0=tp[:], in1=ts2[:], op=ALU.mult))
        vop(lambda e, cf=cf: e.tensor_scalar(out=tp[:], in0=tp[:], scalar1=cf, scalar2=None, op0=ALU.add))
    vop(lambda e: e.tensor_tensor(out=tp[:], in0=tp[:], in1=tsv[:], op=ALU.mult))
    vop(lambda e: e.tensor_scalar(out=trl[:], in0=lam, scalar1=-1.0, scalar2=0.0, op0=ALU.mult, op1=ALU.max))
    vop(lambda e: e.scalar_tensor_tensor(out=trl[:], in0=tp[:], scalar=2.0, in1=trl[:], op0=ALU.mult, op1=ALU.add))
    vop(lambda e: e.tensor_scalar(out=CF[:, :, 0:2], in0=trl[:], scalar1=-4.0, scalar2=None, op0=ALU.mult), writes=[r_CF])
    vop(lambda e: e.tensor_scalar(out=CF[:, :, 2:4], in0=trl[:], scalar1=-8.0, scalar2=None, op0=ALU.mult), writes=[r_CF])

    hv = h.rearrange("(kc p) n -> p kc n", p=128)
    pi = 0
    last_pe = None
    for bi, (c0, c1) in enumerate(TBLK):
        w = c1 - c0
        ht = HT[bi % 2]
        rht = r_HT[bi % 2]
        s.dma([lambda e, ht=ht, c0=c0, c1=c1, w=w, q=q: e.dma_start(out=ht[:, 4 * q:4 * q + 4, :w], in_=hv[:, 4 * q:4 * q + 4, c0:c1])
               for q in range(4)], writes=[rht])
        for m in range(4):
            ps = PS[pi % 4]
            rp = r_PS[pi % 4]
            pi += 1
            for kc in range(KC):
                last_pe = s.op('tensor', lambda e, ps=ps, ht=ht, m=m, kc=kc, w=w: e.matmul(
                    ps[:, :w], lhsT=WB[:, kc, m * 128:(m + 1) * 128], rhs=ht[:, kc, :w],
                    start=(kc == 0), stop=(kc == KC - 1)), reads=[r_WBq[kc // 2], rht], writes=[rp])
            if m < 2:
                s.op('scalar', lambda e, ps=ps, m=m, c0=c0, c1=c1, w=w: e.activation(
                    out=G[:, m, c0:c1], in_=ps[:, :w], func=AF.Gelu_apprx_tanh), reads=[rp], writes=[r_G])
            else:
                rc = rec_col(c0)
                s.op('vector', lambda e, ps=ps, m=m, rc=rc, w=w: e.tensor_copy(
                    out=REC[:, m - 2, rc:rc + w], in_=ps[:, :w]), reads=[rp], writes=[r_REC])

    def tmp(i, dt=F32):
        ap = AR[:, 512 * i:512 * (i + 1)]
        return ap if dt == F32 else ap.bitcast(BF16)
    ti = [0]

    def nt(dt=F32):
        ti[0] += 1
        return tmp(ti[0] - 1, dt)

    def guard():
        r = Res()
        r.r = [last_pe]
        return r
    XC = [[nt(), nt()] for _ in range(2)]
    xcbt = [nt(BF16) for _ in range(2)]
    XCB = [[xcbt[p][:, 0:512], xcbt[p][:, 512:1024]] for p in range(2)]
    THR = [[nt(), nt()] for _ in range(2)]
    THI = [[nt(), nt()] for _ in range(2)]
    AA = [[nt(), nt()] for _ in range(2)]
    HB = [[nt(), nt()] for _ in range(2)]
    zbt = [nt(BF16), nt(BF16)]
    ZB = [zbt[p][:, 0:1024].rearrange("p (k n) -> p k n", k=2) for p in range(2)]
    TS = [nt(), nt()]
    assert ti[0] * 512 <= 13312
    r_XC = [[guard(), guard()] for _ in range(2)]
    r_XCB = [[guard(), guard()] for _ in range(2)]
    r_THR = [[guard(), guard()] for _ in range(2)]
    r_THI = [[guard(), guard()] for _ in range(2)]
    r_AA = [[guard(), guard()] for _ in range(2)]
    r_HB = [[guard(), guard()] for _ in range(2)]
    r_ZB = [guard(), guard()]
    r_TS = [guard(), guard()]
    psn = [0]

    def nextps():
        i = psn[0] % 8
        psn[0] += 1
        return PS[i], r_PS[i]

    def stage_a(d, bi, par):
        c0, c1 = TBLK[bi]
        w = c1 - c0
        rc = rec_col(c0)
        for ch in range(2):
            s.op('scalar', lambda e, ch=ch: e.activation(
                out=XC[par][ch][:, :w], in_=REC[:, ch, rc - 2:rc - 2 + w], func=AF.Identity,
                scale=CW[:, ch, 0:1], bias=CW[:, ch, 4:5]), reads=[r_REC, r_CW], writes=[r_XC[par][ch]])
            for k in (1, 2, 3):
                s.op('vector', lambda e, ch=ch, k=k: e.scalar_tensor_tensor(
                    out=XC[par][ch][:, :w], in0=REC[:, ch, rc - 2 + k:rc - 2 + k + w], scalar=CW[:, ch, k:k + 1],
                    in1=XC[par][ch][:, :w], op0=ALU.mult, op1=ALU.add),
                    reads=[r_REC, r_CW, r_XC[par][ch]], writes=[r_XC[par][ch]])
            s.op('scalar', lambda e, ch=ch: e.activation(
                out=XCB[par][ch][:, :w], in_=XC[par][ch][:, :w], func=AF.Identity),
                reads=[r_XC[par][ch]], writes=[r_XCB[par][ch]])
        pst = {}
        for g in range(2):
            for m in range(2):
                ps, rp = nextps()
                pst[(g, m)] = (ps, rp)
                for kc in range(2):
                    s.op('tensor', lambda e, ps=ps, g=g, m=m, kc=kc: e.matmul(
                        ps[:, :w], lhsT=GWB[:, d * 2 + g, kc, m * 128:(m + 1) * 128], rhs=XCB[par][kc][:, :w],
                        start=(kc == 0), stop=(kc == 1)), reads=[r_GWB, r_XCB[par][kc]], writes=[rp])
        for m in range(2):
            ps, rp = pst[(0, m)]
            s.op('scalar', lambda e, ps=ps, m=m: e.activation(
                out=THR[par][m][:, :w], in_=ps[:, :w], func=AF.Tanh, bias=GVH[:, m, 2 * d:2 * d + 1], scale=0.5),
                reads=[rp, r_GVH], writes=[r_THR[par][m]])
            ps, rp = pst[(1, m)]
            s.op('scalar', lambda e, ps=ps, m=m: e.activation(
                out=THI[par][m][:, :w], in_=ps[:, :w], func=AF.Tanh, bias=GVH[:, m, 2 * d + 1:2 * d + 2], scale=0.5),
                reads=[rp, r_GVH], writes=[r_THI[par][m]])
        for m in range(2):
            s.op('scalar', lambda e, m=m: e.activation(
                out=AA[par][m][:, :w], in_=THR[par][m][:, :w], func=AF.Exp, scale=CF[:, m, d:d + 1], bias=CF[:, m, d:d + 1]),
                reads=[r_THR[par][m], r_CF], writes=[r_AA[par][m]])
            s.op('scalar', lambda e, m=m: e.activation(
                out=THR[par][m][:, :w], in_=THR[par][m][:, :w], func=AF.Exp, scale=CF[:, m, 2 + d:3 + d], bias=CF[:, m, 2 + d:3 + d]),
                reads=[r_THR[par][m], r_CF], writes=[r_THR[par][m]])

    def stage_b(d, bi, par, first, lastw_):
        c0, c1 = TBLK[bi]
        w = c1 - c0
        for m in range(2):
            s.op('vector', lambda e, m=m: e.tensor_scalar(
                out=THR[par][m][:, :w], in0=THR[par][m][:, :w], scalar1=1.0, scalar2=None, op0=ALU.min),
                reads=[r_THR[par][m]], writes=[r_THR[par][m]])
        for m in range(2):
            s.op('scalar', lambda e, m=m: e.activation(
                out=THR[par][m][:, :w], in_=THR[par][m][:, :w], func=AF.Sqrt, bias=onec[:, 0:1], scale=-0.25),
                reads=[r_THR[par][m], r_one], writes=[r_THR[par][m]])

    def stage_b2(d, bi, par, first, lastw_):
        c0, c1 = TBLK[bi]
        w = c1 - c0
        for m in range(2):
            s.op('vector', lambda e, m=m: e.scalar_tensor_tensor(
                out=THR[par][m][:, :w], in0=THI[par][m][:, :w], scalar=1.0, in1=THR[par][m][:, :w],
                op0=ALU.add, op1=ALU.mult), reads=[r_THR[par][m], r_THI[par][m]], writes=[r_THR[par][m]])
            s.op('vector', lambda e, m=m: e.tensor_tensor(
                out=THR[par][m][:, :w], in0=THR[par][m][:, :w], in1=XC[par][m][:, :w], op=ALU.mult),
                reads=[r_THR[par][m], r_XC[par][m]], writes=[r_THR[par][m]])
            hb = HB[par][m]
            hprev = HB[1 - par][m]
            if d == 0:
                init = 0.0 if first else hprev[:, lastw_ - 1:lastw_]
                s.op('vector', lambda e, m=m, hb=hb, init=init: e.tensor_tensor_scan(
                    out=hb[:, :w], data0=AA[par][m][:, :w], data1=THR[par][m][:, :w], initial=init, op0=ALU.mult, op1=ALU.add),
                    reads=[r_AA[par][m], r_THR[par][m], r_HB[1 - par][m]], writes=[r_HB[par][m]])
                s.op('gpsimd', lambda e, m=m, hb=hb: e.tensor_copy(out=Hs[:, m, c0:c1], in_=hb[:, :w]),
                     reads=[r_HB[par][m]], writes=[r_H])
            else:
                init = 0.0 if first else hprev[:, 0:1]
                s.op('vector', lambda e, m=m, hb=hb, init=init: e.tensor_tensor_scan(
                    out=hb[:, :w][:, ::-1], data0=AA[par][m][:, :w][:, ::-1], data1=THR[par][m][:, :w][:, ::-1],
                    initial=init, op0=ALU.mult, op1=ALU.add),
                    reads=[r_AA[par][m], r_THR[par][m], r_HB[1 - par][m]], writes=[r_HB[par][m]])
                zb = ZB[par]
                s.op('gpsimd', lambda e, m=m, hb=hb: e.tensor_tensor(
                    out=TS[par][:, :w], in0=hb[:, :w], in1=Hs[:, m, c0:c1], op=ALU.add),
                    reads=[r_HB[par][m], r_H, r_TS[par]], writes=[r_TS[par]])
                s.op('gpsimd', lambda e, m=m, zb=zb: e.tensor_tensor(
                    out=zb[:, m, :w], in0=TS[par][:, :w], in1=G[:, m, c0:c1], op=ALU.mult),
                    reads=[r_TS[par], r_G], writes=[r_ZB[par]])
        if d == 1:
            zb = ZB[par]
            s.dma([lambda e, zb=zb, m=m: e.dma_start(out=z[m * 128:(m + 1) * 128, c0:c1], in_=zb[:, m, :w]) for m in range(2)],
                  reads=[r_ZB[par]], final=True)

    seq = [(0, bi) for bi in range(17)] + [(1, bi) for bi in [0] + list(range(16, 0, -1))]
    stage_a(seq[0][0], seq[0][1], 0)
    for n, (d, bi) in enumerate(seq):
        first = (n == 0) or (n == 17)
        pw = 0 if first else (TBLK[seq[n - 1][1]][1] - TBLK[seq[n - 1][1]][0])
        stage_b(d, bi, n % 2, first, pw)
        if n + 1 < len(seq):
            stage_a(seq[n + 1][0], seq[n + 1][1], (n + 1) % 2)
        stage_b2(d, bi, n % 2, first, pw)
    return c.finish()


def gather_h_all(hs):
    ctxp = np.concatenate([hk[:, TL:] for hk in hs], axis=1)
    latp = np.concatenate([hk[:, :TL] for hk in hs], axis=1)
    return np.ascontiguousarray(np.concatenate([ctxp, latp], axis=1))


def scatter_z(zs):
    zall = np.concatenate(zs, axis=0)
    out = []
    for k in range(NCORES):
        out.append(np.ascontiguousarray(np.concatenate(
            [zall[:, CTX + k * TL:CTX + (k + 1) * TL], zall[:, k * TC:(k + 1) * TC]], axis=1)))
    return out


def chan_vec(v, k):
    return np.asarray(v[k * 256:(k + 1) * 256], np.float32).reshape(2, 128).T


def lru_inputs(inp, j, k, h_all):
    w_in = inp['lru_w_in'][j]
    win = np.ascontiguousarray(np.concatenate([w_in[:, k * 256:(k + 1) * 256], w_in[:, D + k * 256:D + (k + 1) * 256]], axis=1))
    cw = np.zeros((128, 2, 8), np.float32)
    for t in range(4):
        cw[:, :, t] = chan_vec(inp['lru_conv_w'][j][t], k)
    cw[:, :, 4] = chan_vec(inp['lru_conv_b'][j], k)
    gv = np.zeros((128, 2, 8), np.float32)
    for d in range(2):
        for g in range(2):
            gv[:, :, 2 * d + g] = chan_vec(inp['lru_gate_b'][j][d][g], k)
        gv[:, :, 4 + d] = chan_vec(inp['lru_lambda'][j][d], k)
    gw = np.ascontiguousarray(inp['lru_gate_w'][j][:, :, k])
    return {"h": h_all, "win": win, "cw": cw, "gw": gw, "gv": gv}


def build_c():
    c = Ctx()
    s = c.s
    z = c.dram_in("z", [D, TT], BF16)
    x = c.dram_in("x", [D, TT], F32)
    wout = c.dram_in("wout", [KC, 128, KC * 128], F32)
    vec = c.dram_in("vec", [128, 8, KC], F32)
    wr_d = c.dram_in("wr", [128, KC, NE], F32)
    xo = c.dram_out("xo", [D, TT], F32)
    h2 = c.dram_out("h2", [D, TT], BF16)
    aff = c.dram_out("aff", [NE, TT], F32)

    X = c.sb([128, KC, TT], F32)
    Z = c.sb([128, KC, TT], BF16)
    V = c.sb([128, 8, KC], F32)
    WR = c.sb([128, KC, NE], F32)
    WS = [c.sb([128, KC, 128], F32) for _ in range(2)]
    WBF = [c.sb([128, KC, 128], BF16) for _ in range(2)]
    H2F = c.sb([128, KC, 512], F32)
    H2B = c.sb([128, KC, 512], BF16)
    ones = c.sb([128, 128], F32)
    c.epsb = c.sb([128, 1], F32)
    sqs = [c.sb([128, 512], BF16) for _ in range(2)]
    c.onesb = c.sb([128, 128], BF16)
    tmps = [c.sb([128, 512], F32) for _ in range(2)]
    rstd = c.sb([128, 512], F32)
    gs = c.sb([128, 2, KC], F32)
    E = c.sb([NE, 512], F32)
    RS = c.sb([NE, 512], F32)
    AFF = c.sb([NE, TT], F32)
    PS = [c.ps([128, 512]) for _ in range(4)]
    S = c.ps([128, 512])
    L = c.ps([128, 512])
    SM = c.ps([128, 512])
    r_PS = [Res() for _ in range(4)]
    r_Xm = [Res() for _ in range(KC)]
    r_Z, r_V, r_WR, r_ones, r_S, r_rstd, r_gs, r_L, r_SM, r_E, r_RS, r_AFF, r_H2F, r_H2B = (Res() for _ in range(14))
    r_WS = [Res(), Res()]
    r_WBF = [Res(), Res()]
    r_sq = [Res(), Res()]
    r_tmp = [Res(), Res()]

    s.op('vector', lambda e: e.memset(ones[:], 1.0), writes=[r_ones])
    s.op('vector', lambda e: e.memset(c.epsb[:], EPS), writes=[r_ones])
    s.op('vector', lambda e: e.memset(c.onesb[:], 1.0), writes=[r_ones])
    zerob = c.sb([128, 128], BF16)
    s.op('vector', lambda e: e.memset(zerob[:], 0.0), writes=[r_ones])
    zv = z.rearrange("(kc p) n -> p kc n", p=128)
    xv = x.rearrange("(kc p) n -> p kc n", p=128)
    s.dma([lambda e, q=q: e.dma_start(out=Z[:, 4 * q:4 * q + 4, :], in_=zv[:, 4 * q:4 * q + 4, :]) for q in range(4)],
          writes=[r_Z])
    s.dma(lambda e: e.dma_start(out=V[:], in_=vec), writes=[r_V])
    s.dma(lambda e: e.dma_start(out=WR[:], in_=wr_d), writes=[r_WR])
    for wi, row in ((0, 2), (1, 4)):
        s.op('vector', lambda e, wi=wi, row=row: e.scalar_tensor_tensor(
            out=gs[:, wi, :], in0=V[:, row, :], scalar=1.0, in1=V[:, 0, :], op0=ALU.add, op1=ALU.mult),
            reads=[r_V], writes=[r_gs])

    pi = 0
    for m in range(KC):
        ws, wb = WS[m % 2], WBF[m % 2]
        rws, rwb = r_WS[m % 2], r_WBF[m % 2]
        s.dma(lambda e, ws=ws, m=m: e.dma_start(out=ws[:].rearrange("p k n -> p (k n)"), in_=wout[m]), writes=[rws])
        if m == 0:
            for kc in range(KC):
                s.dma(lambda e, kc=kc: e.dma_start(out=X[:, kc, :], in_=xv[:, kc, :]), writes=[r_Xm[kc]])
        s.op('gpsimd', lambda e, ws=ws, wb=wb: e.tensor_copy(out=wb[:], in_=ws[:]), reads=[rws], writes=[rwb])
        for (lo, hi, which) in BLOCKS:
            w = hi - lo
            ps = PS[pi % 4]
            rp = r_PS[pi % 4]
            pi += 1
            for kc in range(KC):
                s.op('tensor', lambda e, ps=ps, wb=wb, kc=kc, lo=lo, hi=hi, w=w: e.matmul(
                    ps[:, :w], lhsT=wb[:, kc, :], rhs=Z[:, kc, lo:hi], start=(kc == 0), stop=(kc == KC - 1)),
                    reads=[rwb, r_Z], writes=[rp])
            g1row = 5 if which == 'l' else 6
            s.op('vector', lambda e, ps=ps, m=m, lo=lo, hi=hi, w=w, g1row=g1row: e.scalar_tensor_tensor(
                out=X[:, m, lo:hi], in0=ps[:, :w], scalar=V[:, g1row, m:m + 1], in1=X[:, m, lo:hi],
                op0=ALU.mult, op1=ALU.add), reads=[rp, r_V, r_Xm[m]], writes=[r_Xm[m]])

    xov = xo.rearrange("(kc p) n -> p kc n", p=128)
    for m in range(KC):
        s.dma(lambda e, m=m: e.dma_start(out=xov[:, m, :], in_=X[:, m, :]), reads=[r_Xm[m]], final=True)
    h2v = h2.rearrange("(kc p) n -> p kc n", p=128)
    ti = 0
    for (lo, hi, which) in BLOCKS:
        w = hi - lo
        emit_rstd(c, s, X, r_Xm, ones, r_ones, sqs, r_sq, S, r_S, rstd, r_rstd, lo, hi)
        wi = 0 if which == 'l' else 1
        shrow = 1 if which == 'l' else 3
        for kc in range(KC):
            tmp = tmps[ti % 2]
            rt = r_tmp[ti % 2]
            ti += 1
            s.op('vector', lambda e, tmp=tmp, kc=kc, wi=wi, lo=lo, hi=hi, w=w: e.scalar_tensor_tensor(
                out=tmp[:, :w], in0=X[:, kc, lo:hi], scalar=gs[:, wi, kc:kc + 1], in1=rstd[:, :w],
                op0=ALU.mult, op1=ALU.mult), reads=[r_Xm[kc], r_gs, r_rstd], writes=[rt])
            s.op('scalar', lambda e, tmp=tmp, kc=kc, shrow=shrow, w=w: e.activation(
                out=H2F[:, kc, :w], in_=tmp[:, :w], func=AF.Identity, bias=V[:, shrow, kc:kc + 1], scale=1.0),
                reads=[rt, r_V], writes=[r_H2F])
            s.op('scalar', lambda e, tmp=tmp, kc=kc, shrow=shrow, w=w: e.activation(
                out=H2B[:, kc, :w], in_=tmp[:, :w], func=AF.Identity, bias=V[:, shrow, kc:kc + 1], scale=1.0),
                reads=[rt, r_V], writes=[r_H2B])
            s.op('tensor', lambda e, kc=kc, w=w: e.matmul(
                L[:NE, :w], lhsT=WR[:, kc, :], rhs=H2F[:, kc, :w], start=(kc == 0), stop=(kc == KC - 1)),
                reads=[r_WR, r_H2F], writes=[r_L])
        s.dma([lambda e, q=q, lo=lo, hi=hi, w=w: e.dma_start(out=h2v[:, 4 * q:4 * q + 4, lo:hi], in_=H2B[:, 4 * q:4 * q + 4, :w])
               for q in range(4)], reads=[r_H2B], final=True)
        s.op('tensor', lambda e, w=w: e.matmul(L[:NE, :w], lhsT=zerob[:, :NE], rhs=sqs[0][:, :w], start=False, stop=True),
             reads=[r_sq[0]], writes=[r_L])
        s.op('scalar', lambda e, w=w: e.activation(out=E[:, :w], in_=L[:NE, :w], func=AF.Exp), reads=[r_L], writes=[r_E])
        s.op('tensor', lambda e, w=w: e.matmul(SM[:NE, :w], lhsT=ones[:NE, :NE], rhs=E[:, :w], start=True, stop=False),
             reads=[r_E, r_ones], writes=[r_SM])
        s.op('tensor', lambda e, w=w: e.matmul(SM[:NE, :w], lhsT=zerob[:, :NE], rhs=sqs[0][:, :w], start=False, stop=True),
             reads=[r_sq[0]], writes=[r_SM])
        s.op('vector', lambda e, w=w: e.reciprocal(out=RS[:, :w], in_=SM[:NE, :w]), reads=[r_SM], writes=[r_RS])
        s.op('vector', lambda e, w=w, lo=lo, hi=hi: e.tensor_tensor(out=AFF[:, lo:hi], in0=E[:, :w], in1=RS[:, :w], op=ALU.mult),
             reads=[r_E, r_RS], writes=[r_AFF])
    s.dma(lambda e: e.dma_start(out=aff, in_=AFF[:]), reads=[r_AFF], final=True)
    return c.finish()


NIT = 26
BIG = float(1 << 20)


def build_d():
    c = Ctx()
    s = c.s
    al_d = c.dram_in("al", [128, 128], F32)
    ac_d = c.dram_in("ac", [128, 128], F32)
    h2tok = c.dram_in("h2tok", [NTOK, D], BF16)
    wg_d = c.dram_in("wg", [2, 8, 128, KC * 128], F32)
    wu_d = c.dram_in("wu", [2, 8, 128, KC * 128], F32)
    wd_d = c.dram_in("wd", [2, 4, 128, 8 * 512], F32)
    identb_d = c.dram_in("identb", [128, 128], BF16)
    tri_d = c.dram_in("tri", [128, 128], BF16)
    blk_d = c.dram_in("blk", [128, 128], BF16)
    iota_d = c.dram_in("iota", [128, 1024], F32)
    sel_d = c.dram_in("sel", [128, 2], BF16)
    ebm_d = c.dram_in("ebm", [128, 2], F32)
    y = c.dram_out("y", [2, SLOTS, D], BF16)
    ridx = c.dram_out("ridx", [128, 2, 128], I32)

    WG = c.sb([128, KC, FF], BF16)
    WU = c.sb([128, KC, FF], BF16)
    WD = c.sb([128, 8, D], BF16)
    STG = [c.sb([128, 2048], F32) for _ in range(3)]
    XS = c.sb([128, KC, SLOTS], BF16)
    UT = c.sb([128, 8, SLOTS], BF16)
    XG = [c.sb([128, D], BF16) for _ in range(2)]
    YT = [c.sb([128, 512], BF16) for _ in range(4)]
    SGT = [c.sb([128, 512], F32) for _ in range(2)]
    IOTA = c.sb([128, 1024], F32)
    IND = [c.sb([128, 1024], BF16) for _ in range(2)]
    identb = c.sb([128, 128], BF16)
    TRI = c.sb([128, 128], BF16)
    BLK = c.sb([128, 128], BF16)
    SEL = c.sb([128, 2], BF16)
    SELH = c.sb([128, 2], BF16)
    NB = c.sb([128, 128], F32)
    INDA = [c.sb([128, 1024], BF16) for _ in range(2)]
    r_NB = Res()
    r_INDA = [Res(), Res()]
    EBM = c.sb([128, 2], F32)
    ONES = c.sb([128, 128], F32)
    A = [c.sb([128, 128], F32) for _ in range(2)]
    M_ = c.sb([128, 128], F32)
    CS = c.sb([128, 128], F32)
    JUNK = c.sb([128, 128], F32)
    RIF = c.sb([128, 128], F32)
    RII = c.sb([128, 2, 128], I32)
    mid = c.sb([128, 1], F32)
    cntp = c.sb([128, 1], BF16)
    tt = c.sb([128, 1], F32)
    tot = c.sb([128, 1], BF16)
    IDXF = c.sb([128, 9, 2], F32)
    IDXI = c.sb([128, 9, 2], I32)
    PS = [c.ps([128, 512]) for _ in range(8)]
    r_PS = [Res() for _ in range(8)]

    (r_A0, r_A1, r_id, r_tri, r_blk, r_iota, r_sel, r_ebm, r_ones, r_M, r_CS, r_junk, r_RIF, r_RII, r_mid, r_cntp,
     r_tt, r_tot, r_IDXF, r_IDXI, r_WG, r_WU, r_WD, r_XS, r_UT) = (Res() for _ in range(25))
    r_A = [r_A0, r_A1]
    r_STG = [Res() for _ in range(3)]
    r_XG = [Res(), Res()]
    r_YT = [Res() for _ in range(4)]
    r_WGm = [Res() for _ in range(8)]
    r_WUm = [Res() for _ in range(8)]
    r_WDn = [Res() for _ in range(4)]
    r_SGT = [Res(), Res()]
    r_IND = [Res(), Res()]

    for (t, d_, r) in ((A[0], al_d, r_A0), (A[1], ac_d, r_A1), (identb, identb_d, r_id), (TRI, tri_d, r_tri),
                       (BLK, blk_d, r_blk), (IOTA, iota_d, r_iota), (SEL, sel_d, r_sel), (EBM, ebm_d, r_ebm)):
        s.dma(lambda e, t=t, d_=d_: e.dma_start(out=t[:], in_=d_), writes=[r])
    s.op('vector', lambda e: e.memset(ONES[:], 1.0), writes=[r_ones])
    s.op('vector', lambda e: e.tensor_scalar(out=SELH[:], in0=SEL[:], scalar1=0.5, scalar2=None, op0=ALU.mult),
         reads=[r_sel], writes=[r_sel])

    stn = [0]

    def stage_cast(src_ap, dst_ap, rdst):
        st = STG[stn[0] % 3]
        rs_ = r_STG[stn[0] % 3]
        stn[0] += 1
        s.dma(lambda e, st=st, src_ap=src_ap: e.dma_start(out=st[:], in_=src_ap), writes=[rs_])
        return st, rs_

    def load_gu(el, m):
        for (Wt, rWm, src) in ((WG, r_WGm, wg_d), (WU, r_WUm, wu_d)):
            st, rs_ = stage_cast(src[el, m], None, None)
            s.op('scalar', lambda e, st=st, Wt=Wt, m=m: e.activation(
                out=Wt[:, :, m * 128:(m + 1) * 128], in_=st[:].rearrange("p (k n) -> p k n", k=KC), func=AF.Copy),
                reads=[rs_], writes=[rWm[m]])

    def load_d(el, nb):
        for hf in range(2):
            st, rs_ = stage_cast(wd_d[el, nb][:, hf * 2048:(hf + 1) * 2048], None, None)
            s.op('scalar', lambda e, st=st, nb=nb, hf=hf: e.activation(
                out=WD[:, hf * 4:hf * 4 + 4, nb * 512:(nb + 1) * 512], in_=st[:].rearrange("p (m n) -> p m n", m=4),
                func=AF.Copy), reads=[rs_], writes=[r_WDn[nb]])

    def load_weights(el):
        for m in range(8):
            load_gu(el, m)
        for nb in range(4):
            load_d(el, nb)

    load_weights(0)

    mids = [mid, c.sb([128, 1], F32)]
    cntps = [cntp, c.sb([128, 1], BF16)]
    tts = [tt, c.sb([128, 1], F32)]
    junks = [JUNK, c.sb([128, 128], F32)]
    r_mids = [r_mid, Res()]
    r_cntps = [r_cntp, Res()]
    r_tts = [r_tt, Res()]
    r_junks = [r_junk, Res()]
    caps = [CAPL, CAPC]
    bps = [0, 2]
    for si in range(2):
        s.op('vector', lambda e, si=si: e.memset(mids[si][:], 0.5), writes=[r_mids[si]])
    for n in range(NIT):
        wn = 2.0 ** -(n + 1)
        for si in range(2):
            s.op('vector', lambda e, si=si: e.tensor_scalar(out=junks[si][:], in0=A[si][:], scalar1=mids[si][:, 0:1], scalar2=0.0,
                                                           op0=ALU.is_ge, op1=ALU.add, accum_out=cntps[si][:, 0:1]),
                 reads=[r_A[si], r_mids[si]], writes=[r_junks[si], r_cntps[si]])
        for si in range(2):
            s.op('tensor', lambda e, si=si: e.matmul(PS[bps[si]][:, 0:1], lhsT=BLK[:], rhs=cntps[si][:, 0:1], start=True, stop=True),
                 reads=[r_blk, r_cntps[si]], writes=[r_PS[bps[si]]])
        for si in range(2):
            s.op('vector', lambda e, si=si: e.tensor_scalar(out=tts[si][:], in0=PS[bps[si]][:, 0:1], scalar1=float(caps[si]), scalar2=0.5,
                                                           op0=ALU.is_ge, op1=ALU.subtract),
                 reads=[r_PS[bps[si]]], writes=[r_tts[si]])
            s.op('vector', lambda e, si=si, wn=wn: e.scalar_tensor_tensor(out=mids[si][:], in0=tts[si][:], scalar=wn, in1=mids[si][:],
                                                                         op0=ALU.mult, op1=ALU.add),
                 reads=[r_tts[si], r_mids[si]], writes=[r_mids[si]])

    def select(si, cap, nch, mslots):
        At = A[si]
        rA = r_A[si]
        mid = mids[si]
        r_mid = r_mids[si]
        wl = 2.0 ** -(NIT + 1)
        s.op('vector', lambda e: e.tensor_scalar(out=mid[:], in0=mid[:], scalar1=-wl, scalar2=None, op0=ALU.add),
             reads=[r_mid], writes=[r_mid])
        s.op('vector', lambda e: e.tensor_scalar(out=M_[:], in0=At[:], scalar1=mid[:, 0:1], scalar2=None, op0=ALU.is_ge),
             reads=[rA, r_mid], writes=[r_M])
        s.op('vector', lambda e: e.tensor_tensor_scan(out=CS[:], data0=ONES[:], data1=M_[:], initial=0.0,
                                                      op0=ALU.mult, op1=ALU.add),
             reads=[r_M, r_ones], writes=[r_CS])
        s.op('vector', lambda e: e.tensor_copy(out=tot[:], in_=CS[:, 127:128]), reads=[r_CS], writes=[r_tot])
        s.op('tensor', lambda e: e.matmul(PS[1][:, 0:1], lhsT=TRI[:], rhs=tot[:, 0:1], start=True, stop=True),
             reads=[r_tri, r_tot], writes=[r_PS[1]])
        s.op('vector', lambda e: e.tensor_scalar(out=CS[:], in0=CS[:], scalar1=PS[1][:, 0:1], scalar2=None, op0=ALU.add),
             reads=[r_PS[1], r_CS], writes=[r_CS])
        s.op('vector', lambda e: e.tensor_scalar(out=RIF[:], in0=CS[:], scalar1=EBM[:, si:si + 1], scalar2=None, op0=ALU.add),
             reads=[r_CS, r_ebm], writes=[r_RIF])
        s.op('vector', lambda e: e.tensor_tensor(out=RIF[:], in0=RIF[:], in1=M_[:], op=ALU.mult),
             reads=[r_RIF, r_M], writes=[r_RIF])
        s.op('vector', lambda e: e.tensor_scalar(out=RII[:, si, :], in0=RIF[:], scalar1=BIG, scalar2=None, op0=ALU.add),
             reads=[r_RIF], writes=[r_RII])
        use_act = (si == 0)
        if use_act:
            s.op('vector', lambda e: e.tensor_scalar(out=NB[:], in0=CS[:], scalar1=-1.0, scalar2=0.5, op0=ALU.mult, op1=ALU.add),
                 reads=[r_CS], writes=[r_NB])
        for j in range(128):
            on_act = use_act and (j % 2 == 1)
            ind = INDA[(j // 2) % 2] if on_act else IND[(j // 2) % 2 if use_act else j % 2]
            rind = r_INDA[(j // 2) % 2] if on_act else r_IND[(j // 2) % 2 if use_act else j % 2]
            if on_act:
                s.op('scalar', lambda e, ind=ind, j=j: e.activation(
                    out=ind[:, :nch * mslots], in_=IOTA[:, :nch * mslots], func=AF.Sign, bias=NB[:, j:j + 1], scale=1.0),
                    reads=[r_iota, r_NB], writes=[rind])
            else:
                s.op('vector', lambda e, ind=ind, j=j: e.tensor_scalar(
                    out=ind[:, :nch * mslots], in0=IOTA[:, :nch * mslots], scalar1=CS[:, j:j + 1], scalar2=None, op0=ALU.is_ge),
                    reads=[r_iota, r_CS], writes=[rind])
            selm = SELH if on_act else SEL
            for sc in range(nch):
                s.op('tensor', lambda e, ind=ind, sc=sc, j=j, selm=selm: e.matmul(
                    PS[sc][:mslots, 0:2], lhsT=ind[:, sc * mslots:(sc + 1) * mslots], rhs=selm[:],
                    start=(j == 0), stop=(j == 127)), reads=[rind, r_sel], writes=[r_PS[sc]])
        for sc in range(nch):
            dst = sc if si == 0 else 8
            off = float(SEQ // 4) if si == 0 else float(SEQ)
            s.op('vector', lambda e, sc=sc, dst=dst, off=off: e.tensor_scalar(
                out=IDXF[:mslots, dst, :], in0=PS[sc][:mslots, 0:2], scalar1=off, scalar2=None, op0=ALU.add),
                reads=[r_PS[sc]], writes=[r_IDXF])

    select(0, CAPL, 8, 128)
    select(1, CAPC, 1, 32)
    s.op('vector', lambda e: e.tensor_copy(out=IDXI[:], in_=IDXF[:]), reads=[r_IDXF], writes=[r_IDXI])
    s.dma(lambda e: e.dma_start(out=ridx, in_=RII[:]), reads=[r_RII], final=True)

    SBLK = [(0, 512), (512, 1024), (1024, 1056)]
    psn = [0]
    xgn = [0]
    ytn = [0]
    sgn = [0]

    def nextps():
        i = psn[0] % 8
        psn[0] += 1
        return PS[i], r_PS[i]

    def expert(el):
        for sc in range(9):
            P = 128 if sc < 8 else CAPC
            xg = XG[xgn[0] % 2]
            rxg = r_XG[xgn[0] % 2]
            xgn[0] += 1
            s.dma(lambda e, xg=xg, sc=sc, P=P: e.indirect_dma_start(
                out=xg[:P, :], out_offset=None, in_=h2tok,
                in_offset=bass.IndirectOffsetOnAxis(ap=IDXI[:P, sc, el:el + 1], axis=0),
                bounds_check=c.breg(e, NTOK - 1), oob_is_err=False), reads=[r_IDXI], writes=[rxg], eng='gpsimd')
            for hf in range(2):
                ps, rp = nextps()
                psb = ps[:].bitcast(BF16)
                for q in range(8):
                    fc = hf * 8 + q
                    s.op('tensor', lambda e, psb=psb, xg=xg, fc=fc, q=q, P=P: e.transpose(
                        out=psb[:, q * 128:q * 128 + P], in_=xg[:P, fc * 128:(fc + 1) * 128], identity=identb[:P, :P]),
                        reads=[rxg, r_id], writes=[rp])
                s.op('vector', lambda e, psb=psb, hf=hf, sc=sc, P=P: e.tensor_copy(
                    out=XS[:, hf * 8:hf * 8 + 8, sc * 128:sc * 128 + P],
                    in_=psb.rearrange("p (a b) -> p a b", a=8)[:, :, :P]), reads=[rp], writes=[r_XS])
        for m in range(8):
            for (lo, hi) in SBLK:
                w = hi - lo
                pg, rpg = nextps()
                pu, rpu = nextps()
                for kc in range(KC):
                    s.op('tensor', lambda e, pg=pg, m=m, kc=kc, lo=lo, hi=hi, w=w: e.matmul(
                        pg[:, :w], lhsT=WG[:, kc, m * 128:(m + 1) * 128], rhs=XS[:, kc, lo:hi],
                        start=(kc == 0), stop=(kc == KC - 1)), reads=[r_WGm[m], r_XS], writes=[rpg])
                for kc in range(KC):
                    s.op('tensor', lambda e, pu=pu, m=m, kc=kc, lo=lo, hi=hi, w=w: e.matmul(
                        pu[:, :w], lhsT=WU[:, kc, m * 128:(m + 1) * 128], rhs=XS[:, kc, lo:hi],
                        start=(kc == 0), stop=(kc == KC - 1)), reads=[r_WUm[m], r_XS], writes=[rpu])
                sg = SGT[sgn[0] % 2]
                rsg = r_SGT[sgn[0] % 2]
                sgn[0] += 1
                s.op('scalar', lambda e, sg=sg, pg=pg, w=w: e.activation(out=sg[:, :w], in_=pg[:, :w], func=AF.Silu),
                     reads=[rpg], writes=[rsg])
                s.op('vector', lambda e, sg=sg, pu=pu, m=m, lo=lo, hi=hi, w=w: e.tensor_tensor(
                    out=UT[:, m, lo:hi], in0=sg[:, :w], in1=pu[:, :w], op=ALU.mult), reads=[rsg, rpu], writes=[r_UT])
            if el == 0:
                load_gu(1, m)
        for nb in range(4):
            for sc in range(9):
                P = 128 if sc < 8 else CAPC
                yt = YT[ytn[0] % 4]
                ryt = r_YT[ytn[0] % 4]
                ytn[0] += 1
                py, rpy = nextps()
                for m in range(8):
                    s.op('tensor', lambda e, py=py, m=m, sc=sc, nb=nb, P=P: e.matmul(
                        py[:P, :], lhsT=UT[:, m, sc * 128:sc * 128 + P], rhs=WD[:, m, nb * 512:(nb + 1) * 512],
                        start=(m == 0), stop=(m == 7)), reads=[r_UT, r_WDn[nb]], writes=[rpy])
                s.op('scalar', lambda e, py=py, yt=yt, P=P: e.activation(
                    out=yt[:P, :], in_=py[:P, :], func=AF.Copy), reads=[rpy], writes=[ryt])
                s.dma(lambda e, yt=yt, sc=sc, nb=nb, P=P: e.dma_start(
                    out=y[el, sc * 128:sc * 128 + P, nb * 512:(nb + 1) * 512], in_=yt[:P, :]),
                    reads=[ryt], final=True, eng='scalar')
            if el == 0:
                load_d(1, nb)

    expert(0)
    expert(1)
    return c.finish()


def d_consts(k):
    p = np.arange(128)
    same = (p[:, None] // 64) == (p[None, :] // 64)
    tri = (same & (p[:, None] < p[None, :])).astype(np.float32).astype(NPBF)
    blk = same.astype(np.float32).astype(NPBF)
    iota = np.ascontiguousarray(np.broadcast_to(np.arange(1024, dtype=np.float32), (128, 1024)))
    sel = np.stack([(p // 64 == 0), (p // 64 == 1)], axis=1).astype(NPBF)
    ebm = np.zeros((128, 2), np.float32)
    e_glob = 2 * k + p // 64
    ebm[:, 0] = e_glob * SLOTS - 1 - BIG
    ebm[:, 1] = e_glob * SLOTS + CAPL - 1 - BIG
    return {"identb": np.eye(128, dtype=np.float32).astype(NPBF), "tri": tri, "blk": blk, "iota": iota,
            "sel": sel, "ebm": ebm}


def moe_inputs(inp, layer, cres):
    aff_l = np.concatenate([r["aff"][:, :TL] for r in cres], axis=1)
    aff_c = np.concatenate([r["aff"][:, TL:] for r in cres], axis=1)
    h2tok = np.ascontiguousarray(np.concatenate(
        [r["h2"][:, :TL].T for r in cres] + [r["h2"][:, TL:].T for r in cres], axis=0))
    maps = []
    for k in range(NCORES):
        al = np.ascontiguousarray(aff_l[2 * k:2 * k + 2].reshape(128, 128))
        ac = np.full((128, 128), -1.0, np.float32)
        for el in range(2):
            ac[el * 64:el * 64 + 2] = aff_c[2 * k + el].reshape(2, 128)
        def tile_gu(w):
            return np.ascontiguousarray(w.reshape(2, KC, 128, 8, 128).transpose(0, 3, 2, 1, 4).reshape(2, 8, 128, KC * 128))

        def tile_d(w):
            return np.ascontiguousarray(w.reshape(2, 8, 128, 4, 512).transpose(0, 3, 2, 1, 4).reshape(2, 4, 128, 8 * 512))
        m = {"al": al, "ac": ac, "h2tok": h2tok,
             "wg": tile_gu(inp['moe_w_gate'][layer, 2 * k:2 * k + 2]), "wu": tile_gu(inp['moe_w_up'][layer, 2 * k:2 * k + 2]),
             "wd": tile_d(inp['moe_w_down'][layer, 2 * k:2 * k + 2])}
        m.update(d_consts(k))
        maps.append(m)
    return maps, aff_l, aff_c


def combine_inputs(dres, aff_l, aff_c):
    yall = np.ascontiguousarray(np.concatenate([r["y"].reshape(2 * SLOTS, D) for r in dres], axis=0))
    ridx_l = np.concatenate([r["ridx"][:, 0, :].reshape(2, SEQ) for r in dres], axis=0)
    ridx_c = np.concatenate([r["ridx"][:, 1, :].reshape(2, 64, 128)[:, :2].reshape(2, CTX) for r in dres], axis=0)
    out = []
    for k in range(NCORES):
        ri = np.full((128, NTILE, NE), int(BIG), np.int32)
        af = np.zeros((128, NTILE, NE), np.float32)
        ri[:, :8, :] = ridx_l[:, k * TL:(k + 1) * TL].T.reshape(8, 128, NE).transpose(1, 0, 2)
        af[:, :8, :] = aff_l[:, k * TL:(k + 1) * TL].T.reshape(8, 128, NE).transpose(1, 0, 2)
        ri[:TC, 8, :] = ridx_c[:, k * TC:(k + 1) * TC].T
        af[:TC, 8, :] = aff_c[:, k * TC:(k + 1) * TC].T
        out.append((np.ascontiguousarray(ri), np.ascontiguousarray(af)))
    return yall, out


GRID_W = 64
ROWS = SEQ // GRID_W
NEG = -30000.0


def att_pattern(r):
    return 0 if r == 0 else 1 if r == 2 else 3 if r == 124 else 4 if r == 126 else 2


def build_att():
    c = Ctx()
    s = c.s
    h = c.dram_in("h", [D, NTOK], BF16)
    wqkv = c.dram_in("wqkv", [D, 768], F32)
    bt_d = c.dram_in("bt", [128, 2, 5, 5, 128], BF16)
    identb_d = c.dram_in("identb", [128, 128], BF16)
    z = c.dram_out("z", [256, NTOK], BF16)

    QT = c.sb([128, 2, NTOK], BF16)
    KT = c.sb([128, 2, NTOK], BF16)
    VT = c.sb([128, NTOK // 128, 256], BF16)
    WB = c.sb([128, KC, 768], BF16)
    STG = [c.sb([128, 2, 768], F32) for _ in range(2)]
    HT = [c.sb([128, KC, 512], BF16) for _ in range(2)]
    BT = c.sb([128, 2, 5, 5, 128], BF16)
    identb = c.sb([128, 128], BF16)
    onesb = c.sb([128, 128], BF16)
    PT = [c.sb([128, 896], BF16) for _ in range(2)]
    RD = [c.sb([128, 128], F32) for _ in range(2)]
    ZS = [c.sb([128, 2, 512], BF16) for _ in range(2)]
    PS = [c.ps([128, 512]) for _ in range(8)]
    r_PS = [Res() for _ in range(8)]
    r_QT, r_KT, r_VT, r_WB, r_BT, r_id, r_ones = (Res() for _ in range(7))
    r_STG = [Res(), Res()]
    r_HT = [Res(), Res()]
    r_PT = [Res(), Res()]
    r_RD = [Res(), Res()]
    r_ZS = [Res(), Res()]

    s.dma(lambda e: e.dma_start(out=BT[:], in_=bt_d), writes=[r_BT])
    s.dma(lambda e: e.dma_start(out=identb[:], in_=identb_d), writes=[r_id])
    s.op('vector', lambda e: e.memset(onesb[:], 1.0), writes=[r_ones])
    wv = wqkv.rearrange("(kc p) n -> p kc n", p=128)
    r_WBq = [Res() for _ in range(8)]
    for q in range(8):
        st = STG[q % 2]
        rs_ = r_STG[q % 2]
        s.dma(lambda e, st=st, q=q: e.dma_start(out=st[:], in_=wv[:, 2 * q:2 * q + 2, :]), writes=[rs_])
        s.op('scalar', lambda e, st=st, q=q: e.activation(out=WB[:, 2 * q:2 * q + 2, :], in_=st[:], func=AF.Copy),
             reads=[rs_], writes=[r_WBq[q]])

    hv = h.rearrange("(kc p) n -> p kc n", p=128)
    pi = 0
    scale = float(128 ** -0.5)
    for bi, (c0, c1) in enumerate(TBLK):
        w = c1 - c0
        ht = HT[bi % 2]
        rht = r_HT[bi % 2]
        s.dma([lambda e, ht=ht, c0=c0, c1=c1, w=w, q=q: e.dma_start(out=ht[:, 4 * q:4 * q + 4, :w], in_=hv[:, 4 * q:4 * q + 4, c0:c1])
               for q in range(4)], writes=[rht])
        for m in range(4):
            ps, rp = PS[pi % 8], r_PS[pi % 8]
            pi += 1
            for kc in range(KC):
                s.op('tensor', lambda e, ps=ps, ht=ht, m=m, kc=kc, w=w: e.matmul(
                    ps[:, :w], lhsT=WB[:, kc, m * 128:(m + 1) * 128], rhs=ht[:, kc, :w],
                    start=(kc == 0), stop=(kc == KC - 1)), reads=[r_WBq[kc // 2], rht], writes=[rp])
            if m < 2:
                s.op('scalar', lambda e, ps=ps, m=m, c0=c0, c1=c1, w=w: e.activation(
                    out=QT[:, m, c0:c1], in_=ps[:, :w], func=AF.Copy, scale=scale), reads=[rp], writes=[r_QT])
            else:
                s.op('vector', lambda e, ps=ps, m=m, c0=c0, c1=c1, w=w: e.tensor_copy(
                    out=KT[:, m - 2, c0:c1], in_=ps[:, :w]), reads=[rp], writes=[r_KT])
        for t in range(w // 128):
            ps, rp = PS[pi % 8], r_PS[pi % 8]
            pi += 1
            ti = c0 // 128 + t
            for kc in range(KC):
                s.op('tensor', lambda e, ps=ps, ht=ht, t=t, kc=kc: e.matmul(
                    ps[:, :256], lhsT=ht[:, kc, t * 128:(t + 1) * 128], rhs=WB[:, kc, 512:768],
                    start=(kc == 0), stop=(kc == KC - 1)), reads=[r_WBq[kc // 2], rht], writes=[rp])
            if t % 2 == 0:
                s.op('scalar', lambda e, ps=ps, ti=ti: e.activation(out=VT[:, ti, :], in_=ps[:, :256], func=AF.Copy),
                     reads=[rp], writes=[r_VT])
            else:
                s.op('vector', lambda e, ps=ps, ti=ti: e.tensor_copy(out=VT[:, ti, :], in_=ps[:, :256]),
                     reads=[rp], writes=[r_VT])

    un = [0]

    pend = []

    def unit(qc0, local, zs, rzs, zoff):
        for hh in range(2):
            u = un[0]
            un[0] += 1
            pend.append(unit_qk(u, hh, qc0, local, zs, rzs, zoff))
            if len(pend) > 1:
                unit_pv(*pend.pop(0))

    def flush_units():
        while pend:
            unit_pv(*pend.pop(0))

    def unit_qk(u, hh, qc0, local, zs, rzs, zoff):
        if True:
            sa, rsa = PS[(u % 2) * 4 + 0], r_PS[(u % 2) * 4 + 0]
            sb_, rsb = PS[(u % 2) * 4 + 1], r_PS[(u % 2) * 4 + 1]
            po, rpo = PS[(u % 2) * 4 + 2], r_PS[(u % 2) * 4 + 2]
            pd, rpd = PS[(u % 2) * 4 + 3], r_PS[(u % 2) * 4 + 3]
            pt, rpt = PT[u % 2], r_PT[u % 2]
            rd, rrd = RD[u % 2], r_RD[u % 2]
            chunks = []
            if local is not None:
                kc0, pat = local
                for j in range(5):
                    tgt = (sa[:, j * 128:(j + 1) * 128], rsa) if j < 4 else (sb_[:, 0:128], rsb)
                    chunks.append((tgt[0], tgt[1], kc0 + j * 128, (kc0 + j * 128) // 128, (pat, j)))
                chunks.append((sb_[:, 128:256], rsb, 0, 0, None))
                chunks.append((sb_[:, 256:384], rsb, 128, 1, None))
                nA, nB = 512, 384
            else:
                chunks.append((sa[:, 0:128], rsa, 0, 0, None))
                chunks.append((sa[:, 128:256], rsa, 128, 1, None))
                nA, nB = 256, 0
            for (ap, rr, kcol, vt, bias) in chunks:
                s.op('tensor', lambda e, ap=ap, kcol=kcol, hh=hh, bias=bias: e.matmul(
                    ap, lhsT=KT[:, hh, kcol:kcol + 128], rhs=QT[:, hh, qc0:qc0 + 128], start=True, stop=(bias is None)),
                    reads=[r_KT, r_QT], writes=[rr])
                if bias is not None:
                    s.op('tensor', lambda e, ap=ap, hh=hh, bias=bias: e.matmul(
                        ap, lhsT=identb[:], rhs=BT[:, hh, bias[0], bias[1], :], start=False, stop=True),
                        reads=[r_id, r_BT], writes=[rr])
            s.op('scalar', lambda e, sa=sa, pt=pt, nA=nA: e.activation(out=pt[:, 0:nA], in_=sa[:, 0:nA], func=AF.Exp),
                 reads=[rsa], writes=[rpt])
            if nB:
                s.op('scalar', lambda e, sb_=sb_, pt=pt, nB=nB: e.activation(out=pt[:, 512:512 + nB], in_=sb_[:, 0:nB], func=AF.Exp),
                     reads=[rsb], writes=[rpt])
            return (hh, chunks, po, rpo, pd, rpd, pt, rpt, rd, rrd, zs, rzs, zoff)

    def unit_pv(hh, chunks, po, rpo, pd, rpd, pt, rpt, rd, rrd, zs, rzs, zoff):
        if True:
            nchk = len(chunks)
            for ci, (ap, rr, kcol, vt, bias) in enumerate(chunks):
                pcol = ci * 128 if ci < 4 else 512 + (ci - 4) * 128
                s.op('tensor', lambda e, po=po, vt=vt, hh=hh, pt=pt, pcol=pcol, ci=ci, nchk=nchk: e.matmul(
                    po[:, 0:128], lhsT=VT[:, vt, hh * 128:(hh + 1) * 128], rhs=pt[:, pcol:pcol + 128],
                    start=(ci == 0), stop=(ci == nchk - 1)), reads=[r_VT, rpt], writes=[rpo])
            for ci in range(nchk):
                pcol = ci * 128 if ci < 4 else 512 + (ci - 4) * 128
                s.op('tensor', lambda e, pd=pd, pt=pt, pcol=pcol, ci=ci, nchk=nchk: e.matmul(
                    pd[:, 0:128], lhsT=onesb[:], rhs=pt[:, pcol:pcol + 128],
                    start=(ci == 0), stop=(ci == nchk - 1)), reads=[r_ones, rpt], writes=[rpd])
            s.op('vector', lambda e, rd=rd, pd=pd: e.reciprocal(out=rd[:], in_=pd[:, 0:128]), reads=[rpd], writes=[rrd])
            s.op('vector', lambda e, rd=rd, po=po, hh=hh: e.tensor_tensor(
                out=zs[:, hh, zoff:zoff + 128], in0=po[:, 0:128], in1=rd[:], op=ALU.mult),
                reads=[rpo, rrd], writes=[rzs])

    zn = 0
    zs, rzs = ZS[zn % 2], r_ZS[zn % 2]
    zn += 1
    for qt in range(2):
        unit(qt * 128, None, zs, rzs, qt * 128)
    pending_store = [lambda zs=zs, rzs=rzs: s.dma(
        [lambda e, zs=zs, hh=hh: e.dma_start(out=z[hh * 128:(hh + 1) * 128, 0:256], in_=zs[:, hh, 0:256]) for hh in range(2)],
        reads=[rzs], final=True)]
    for rp4 in range(16):
        zs, rzs = ZS[zn % 2], r_ZS[zn % 2]
        zn += 1
        for i in range(4):
            r = 2 * (rp4 * 4 + i)
            base = min(max(r - 4, 0), 118)
            unit(CTX + r * GRID_W, (CTX + base * GRID_W, att_pattern(r)), zs, rzs, i * 128)
            if pending_store:
                pending_store.pop()()
        c0 = CTX + rp4 * 512
        pending_store.append(lambda zs=zs, rzs=rzs, c0=c0: s.dma(
            [lambda e, zs=zs, hh=hh, c0=c0: e.dma_start(out=z[hh * 128:(hh + 1) * 128, c0:c0 + 512], in_=zs[:, hh, :]) for hh in range(2)],
            reads=[rzs], final=True))
    flush_units()
    pending_store.pop()()
    return c.finish()


def att_bias_tiles(rpb2):
    out = np.full((2, 5, 640, 128), NEG, np.float32)
    cq = np.arange(GRID_W)
    cs = np.clip(cq - 8, 0, GRID_W - 16)
    for pat, (r, base) in enumerate(((0, 0), (2, 0), (4, 0), (124, 118), (126, 118))):
        for i in range(2):
            rs = min(max(r + i - 4, 0), ROWS - 8)
            for kr in range(10):
                ar = base + kr
                if not (rs <= ar < rs + 8):
                    continue
                dr = ar - (r + i) + 7
                for c_ in range(GRID_W):
                    kcs = np.arange(cs[c_], cs[c_] + 16)
                    out[:, pat, kr * 64 + kcs, i * 64 + c_] = rpb2[:, dr, kcs - c_ + 15]
    out = out.reshape(2, 5, 5, 128, 128).transpose(3, 0, 1, 2, 4)
    return np.ascontiguousarray(out).astype(NPBF)


def att_inputs(inp, j, k, h_all):
    w = inp['attn_w_qkv'][j]
    wqkv = np.ascontiguousarray(np.concatenate(
        [w[:, i * D + k * 256:i * D + (k + 1) * 256] for i in range(3)], axis=1))
    return {"h": h_all, "wqkv": wqkv, "bt": att_bias_tiles(inp['attn_rpb'][j][2 * k:2 * k + 2]),
            "identb": np.eye(128, dtype=np.float32).astype(NPBF)}


_PROGS = {}


def _prog(name, builder):
    if name not in _PROGS:
        _PROGS[name] = builder()
    return _PROGS[name]


def kernel(**inputs):
    inp = {k: np.asarray(v) for k, v in inputs.items()}
    mods = run_ada(inp)
    x = inp['x'][0]
    ctx = inp['ctx'][0]
    XT = [np.ascontiguousarray(np.concatenate([x[k * TL:(k + 1) * TL].T, ctx[k * TC:(k + 1) * TC].T], axis=1))
          for k in range(NCORES)]
    identb = np.eye(128, dtype=np.float32).astype(NPBF)
    comb = None
    prev_g2 = None
    for i in range(DEPTH):
        j = i // 2
        ml = mods[i, 0].reshape(6, D)
        mc = mods[i, 1].reshape(6, D)
        rows = {0: inp['norm_mix_g'][i], 1: ml[0], 2: ml[1], 3: mc[0], 4: mc[1]}
        if comb is None:
            vec = pack_vec(rows)
            res = run(_prog('t1n', lambda: build_t1('norm', False)), [{"x": XT[k], "vec": vec} for k in range(NCORES)])
        else:
            rows[5], rows[6] = prev_g2
            vec = pack_vec(rows)
            yall, tiles = comb
            res = run(_prog('t1c', lambda: build_t1('norm', True)),
                      [{"x": XT[k], "vec": vec, "yall": yall, "ridx": tiles[k][0], "afft": tiles[k][1], "identb": identb}
                       for k in range(NCORES)])
            XT = [r["xo"] for r in res]
        h_all = gather_h_all([r["h"] for r in res])
        if i % 2 == 0:
            zres = run(_prog('lru', build_lru), [lru_inputs(inp, j, k, h_all) for k in range(NCORES)])
            wout = tile_wout(inp['lru_w_out'][j])
        else:
            zres = run(_prog('att', build_att), [att_inputs(inp, j, k, h_all) for k in range(NCORES)])
            wout = tile_wout(inp['attn_w_out'][j])
        zs = scatter_z([r["z"] for r in zres])
        vec2 = pack_vec({0: inp['norm_ffn_g'][i], 1: ml[3], 2: ml[4], 3: mc[3], 4: mc[4], 5: ml[2], 6: mc[2]})
        wr = np.ascontiguousarray(inp['moe_router'][i].reshape(KC, 128, NE).transpose(1, 0, 2))
        cres = run(_prog('c', build_c), [{"z": zs[k], "x": XT[k], "wout": wout, "vec": vec2, "wr": wr}
                                          for k in range(NCORES)])
        XT = [r["xo"] for r in cres]
        maps, aff_l, aff_c = moe_inputs(inp, i, cres)
        dres = run(_prog('d', build_d), maps)
        comb = combine_inputs(dres, aff_l, aff_c)
        prev_g2 = (ml[5], mc[5])
    rows = {0: inp['final_norm_g'], 5: prev_g2[0], 6: prev_g2[1]}
    vec = pack_vec(rows)
    yall, tiles = comb
    res = run(_prog('t1f', lambda: build_t1('final', True)),
              [{"x": XT[k], "vec": vec, "yall": yall, "ridx": tiles[k][0], "afft": tiles[k][1], "identb": identb}
               for k in range(NCORES)])
    out = np.concatenate([r["o"].T for r in res], axis=0)[None]
    return np.ascontiguousarray(out.astype(np.float32))
```
